# Optimizing a Trainium2 kernel written in Bass

```python
import math
import jax, jax.numpy as jnp
from jax import lax
import numpy as np

D_MODEL = 1024
BATCH = 8
SEQ = 4096
DEPTH = 1

HEAD_DIM = 64
N_HEADS_MOBA = 8
N_HEADS_SB = 8
W_MOBA = N_HEADS_MOBA * HEAD_DIM
W_SB = N_HEADS_SB * HEAD_DIM
MOBA_BLOCK = 256
MOBA_TOPK = 3
MOBA_QCHUNK = 32
SB_QBLOCK = 128
REL_BUCKETS = 32
REL_MAX_DIST = 1024
N_GROUPS = 4
EXPERTS_PER_GROUP = 8
N_EXPERTS = N_GROUPS * EXPERTS_PER_GROUP
EXPERT_TOPK = 2
D_EXPERT = 512
MOE_ROW_BLOCK = 256
N_MOD = 6
RMS_EPS = 1e-6
IN_COLS = 3 * W_MOBA + 3 * W_SB + 2 * D_MODEL

kernel_name = "hybrid_moba_stickbreak_hmoe_block"


def rmsnorm(x, g):
    xf = x.astype(jnp.float32)
    y = xf * lax.rsqrt(jnp.mean(xf * xf, axis=-1, keepdims=True) + RMS_EPS)
    return (y * g.astype(jnp.float32)).astype(x.dtype)


def modulate(h, shift, scale):
    return h * (1 + scale) + shift


def rel_bucket(dist):
    n = jnp.maximum(dist, 0)
    max_exact = REL_BUCKETS // 2
    nf = jnp.maximum(n, 1).astype(jnp.float32)
    large = max_exact + (jnp.log(nf / max_exact) / math.log(REL_MAX_DIST / max_exact)
                         * (REL_BUCKETS - max_exact)).astype(jnp.int32)
    large = jnp.minimum(large, REL_BUCKETS - 1)
    return jnp.where(n < max_exact, n, large)


def moba_attention(q, k, v, rel_bias):
    B, H, S, Dh = q.shape
    n_blk = -(-S // MOBA_BLOCK)
    s_pad = n_blk * MOBA_BLOCK
    n_sel = min(MOBA_TOPK, n_blk)
    pad = ((0, 0), (0, 0), (0, s_pad - S), (0, 0))
    q, k, v = jnp.pad(q, pad), jnp.pad(k, pad), jnp.pad(v, pad)
    kb = k.reshape(B, H, n_blk, MOBA_BLOCK, Dh)
    vb = v.reshape(B, H, n_blk, MOBA_BLOCK, Dh)
    k_mean = jnp.mean(kb, axis=3)
    scale = Dh ** -0.5
    bi = jnp.arange(B)[:, None, None, None]
    hi = jnp.arange(H)[None, :, None, None]
    blk_ids = jnp.arange(n_blk)
    j_ids = jnp.arange(MOBA_BLOCK)

    def chunk(ci):
        q0 = ci * MOBA_QCHUNK
        own = q0 // MOBA_BLOCK
        qc = lax.dynamic_slice_in_dim(q, q0, MOBA_QCHUNK, axis=2)
        t = q0 + jnp.arange(MOBA_QCHUNK)
        gate = jnp.einsum('bhqd,bhnd->bhqn', qc, k_mean).astype(jnp.float32)
        gate = jnp.where(blk_ids < own, gate, -jnp.inf)
        _, sel = lax.top_k(gate, n_sel)
        slot_ok = jnp.arange(n_sel) < own
        k_sel = kb[bi, hi, sel]
        v_sel = vb[bi, hi, sel]
        s_sel = sel[..., None] * MOBA_BLOCK + j_ids
        bias_sel = rel_bias[hi[..., None], rel_bucket(t[None, None, :, None, None] - s_sel)]
        l_sel = jnp.einsum('bhqd,bhqkjd->bhqkj', qc, k_sel).astype(jnp.float32) * scale + bias_sel
        l_sel = jnp.where(slot_ok[:, None], l_sel, -jnp.inf)
        k_own = lax.dynamic_slice_in_dim(k, own * MOBA_BLOCK, MOBA_BLOCK, axis=2)
        v_own = lax.dynamic_slice_in_dim(v, own * MOBA_BLOCK, MOBA_BLOCK, axis=2)
        dist_own = t[:, None] - (own * MOBA_BLOCK + j_ids)[None, :]
        bias_own = rel_bias[:, rel_bucket(dist_own)]
        l_own = jnp.einsum('bhqd,bhjd->bhqj', qc, k_own).astype(jnp.float32) * scale + bias_own
        l_own = jnp.where(dist_own >= 0, l_own, -jnp.inf)
        logits = jnp.concatenate([l_sel.reshape(B, H, MOBA_QCHUNK, n_sel * MOBA_BLOCK), l_own], axis=-1)
        p = jax.nn.softmax(logits, axis=-1)
        p_sel = p[..., :n_sel * MOBA_BLOCK].reshape(B, H, MOBA_QCHUNK, n_sel, MOBA_BLOCK).astype(v.dtype)
        p_own = p[..., n_sel * MOBA_BLOCK:].astype(v.dtype)
        return (jnp.einsum('bhqkj,bhqkjd->bhqd', p_sel, v_sel)
                + jnp.einsum('bhqj,bhjd->bhqd', p_own, v_own))

    outs = lax.map(chunk, jnp.arange(s_pad // MOBA_QCHUNK))
    return outs.transpose(1, 2, 0, 3, 4).reshape(B, H, s_pad, Dh)[:, :, :S]


def stick_breaking_attention(q, k, v):
    B, H, S, Dh = q.shape
    scale = Dh ** -0.5
    s_pos = jnp.arange(S)

    def block(bq):
        q0 = bq * SB_QBLOCK
        qc = lax.dynamic_slice_in_dim(q, q0, SB_QBLOCK, axis=2)
        t = q0 + jnp.arange(SB_QBLOCK)
        z = jnp.einsum('bhqd,bhsd->bhqs', qc, k).astype(jnp.float32) * scale
        past = s_pos[None, :] < t[:, None]
        log_1m = jnp.where(past, jax.nn.log_sigmoid(-z), 0.0)
        after = lax.cumsum(log_1m, axis=3, reverse=True) - log_1m
        a = jnp.where(past, jnp.exp(jax.nn.log_sigmoid(z) + after), 0.0)
        return jnp.einsum('bhqs,bhsd->bhqd', a.astype(v.dtype), v)

    outs = lax.map(block, jnp.arange(S // SB_QBLOCK))
    return outs.transpose(1, 2, 0, 3, 4).reshape(B, H, S, Dh)


def hierarchical_moe(h, w_rg, b_rg, w_re, b_re, w_gate, w_up, w_down):
    B, S, D = h.shape
    N = B * S
    hf = h.reshape(N, D)
    g_prob = jax.nn.softmax((hf @ w_rg + b_rg).astype(jnp.float32), axis=-1)
    g_w, g_idx = lax.top_k(g_prob, 1)
    e_logits = (hf @ w_re + b_re).astype(jnp.float32).reshape(N, N_GROUPS, EXPERTS_PER_GROUP)
    e_logits = jnp.take_along_axis(e_logits, g_idx[:, :, None], axis=1)[:, 0]
    e_top, e_local = lax.top_k(e_logits, EXPERT_TOPK)
    e_w = jax.nn.softmax(e_top, axis=-1) * g_w
    e_id = g_idx * EXPERTS_PER_GROUP + e_local
    n_asg = N * EXPERT_TOPK
    flat_e = e_id.reshape(-1)
    flat_w = e_w.reshape(-1)
    flat_tok = jnp.repeat(jnp.arange(N, dtype=jnp.int32), EXPERT_TOPK)
    order = jnp.argsort(flat_e)
    se, stok, sw = flat_e[order], flat_tok[order], flat_w[order]
    counts = jnp.zeros(N_EXPERTS, jnp.int32).at[flat_e].add(1)
    start = jnp.cumsum(counts) - counts
    padded = (counts + MOE_ROW_BLOCK - 1) // MOE_ROW_BLOCK * MOE_ROW_BLOCK
    pend = jnp.cumsum(padded)
    pstart = pend - padded
    n_blocks = -(-n_asg // MOE_ROW_BLOCK) + N_EXPERTS
    n_rows = n_blocks * MOE_ROW_BLOCK
    dest = pstart[se] + jnp.arange(n_asg, dtype=jnp.int32) - start[se]
    row_tok = jnp.zeros(n_rows, jnp.int32).at[dest].set(stok)
    row_w = jnp.zeros(n_rows, jnp.float32).at[dest].set(sw)
    blk_e = jnp.minimum(jnp.searchsorted(pend, jnp.arange(n_blocks) * MOE_ROW_BLOCK, side='right'),
                        N_EXPERTS - 1)

    def expert_block(args):
        tok, w, e = args
        xb = hf[tok]
        hid = jax.nn.silu(xb @ w_gate[e]) * (xb @ w_up[e])
        return (hid @ w_down[e]) * w[:, None].astype(xb.dtype)

    y = lax.map(expert_block, (row_tok.reshape(n_blocks, MOE_ROW_BLOCK),
                               row_w.reshape(n_blocks, MOE_ROW_BLOCK), blk_e))
    out = jnp.zeros((N, D), h.dtype).at[row_tok].add(y.reshape(n_rows, D))
    return out.reshape(B, S, D)


def split_heads(t, n_heads):
    B, S, _ = t.shape
    return t.reshape(B, S, n_heads, HEAD_DIM).transpose(0, 2, 1, 3)


def merge_heads(t):
    B, H, S, Dh = t.shape
    return t.transpose(0, 2, 1, 3).reshape(B, S, H * Dh)


def setup_inputs(seed: int = 0) -> dict:
    key = jax.random.key(seed)
    ks = jax.random.split(key, 20)
    f = jnp.float32
    D = D_MODEL
    nrm = lambda k, shape, s: jax.random.normal(k, shape, f) * s
    return {
        "x": nrm(ks[0], (BATCH, SEQ, D), 1.0),
        "c": nrm(ks[1], (BATCH, D), 1.0),
        "w_ada": nrm(ks[2], (DEPTH, D, N_MOD * D), 0.5 * D ** -0.5),
        "b_ada": nrm(ks[3], (DEPTH, N_MOD * D), 0.01),
        "g_mix": 1.0 + nrm(ks[4], (DEPTH, D), 0.02),
        "w_in": nrm(ks[5], (DEPTH, D, IN_COLS), D ** -0.5),
        "g_q": 1.0 + nrm(ks[6], (DEPTH, HEAD_DIM), 0.02),
        "g_k": 1.0 + nrm(ks[7], (DEPTH, HEAD_DIM), 0.02),
        "rel_bias": nrm(ks[8], (N_HEADS_MOBA, REL_BUCKETS), 0.5),
        "w_br_moba": nrm(ks[9], (DEPTH, W_MOBA, D), W_MOBA ** -0.5),
        "w_br_sb": nrm(ks[10], (DEPTH, W_SB, D), W_SB ** -0.5),
        "w_out": nrm(ks[11], (DEPTH, D, D), D ** -0.5),
        "g_ffn": 1.0 + nrm(ks[12], (DEPTH, D), 0.02),
        "w_rg": nrm(ks[13], (DEPTH, D, N_GROUPS), D ** -0.5),
        "b_rg": nrm(ks[14], (DEPTH, N_GROUPS), 0.01),
        "w_re": nrm(ks[15], (DEPTH, D, N_EXPERTS), D ** -0.5),
        "b_re": nrm(ks[16], (DEPTH, N_EXPERTS), 0.01),
        "w_gate": nrm(ks[17], (DEPTH, N_EXPERTS, D, D_EXPERT), D ** -0.5),
        "w_up": nrm(ks[18], (DEPTH, N_EXPERTS, D, D_EXPERT), D ** -0.5),
        "w_down": nrm(ks[19], (DEPTH, N_EXPERTS, D_EXPERT, D), D_EXPERT ** -0.5),
    }


def reference(x, c, w_ada, b_ada, g_mix, w_in, g_q, g_k, rel_bias, w_br_moba, w_br_sb, w_out,
              g_ffn, w_rg, b_rg, w_re, b_re, w_gate, w_up, w_down):
    B, S, D = x.shape
    splits = list(np.cumsum([W_MOBA, W_MOBA, W_MOBA, W_SB, W_SB, W_SB, D_MODEL]))
    for l in range(DEPTH):
        mod = (jax.nn.silu(c) @ w_ada[l] + b_ada[l]).reshape(B, N_MOD, 1, D)
        sh_a, sc_a, gt_a, sh_f, sc_f, gt_f = (mod[:, i] for i in range(N_MOD))
        h = modulate(rmsnorm(x, g_mix[l]), sh_a, sc_a)
        proj = h @ w_in[l]
        qa, ka, va, qb, kb, vb, gate_a, gate_b = jnp.split(proj, splits, axis=-1)
        qa = rmsnorm(split_heads(qa, N_HEADS_MOBA), g_q[l])
        ka = rmsnorm(split_heads(ka, N_HEADS_MOBA), g_k[l])
        o_a = merge_heads(moba_attention(qa, ka, split_heads(va, N_HEADS_MOBA), rel_bias))
        o_b = merge_heads(stick_breaking_attention(split_heads(qb, N_HEADS_SB),
                                                   split_heads(kb, N_HEADS_SB),
                                                   split_heads(vb, N_HEADS_SB)))
        merged = (jax.nn.sigmoid(gate_a) * (o_a @ w_br_moba[l])
                  + jax.nn.sigmoid(gate_b) * (o_b @ w_br_sb[l]))
        x = x + gt_a * (merged @ w_out[l])
        h2 = modulate(rmsnorm(x, g_ffn[l]), sh_f, sc_f)
        x = x + gt_f * hierarchical_moe(h2, w_rg[l], b_rg[l], w_re[l], b_re[l],
                                        w_gate[l], w_up[l], w_down[l])
    return x
```

```python
import math
from contextlib import ExitStack
import numpy as np
import ml_dtypes
import concourse.bass as bass
import concourse.mybir as mybir
from concourse.bass_utils import run_bass_kernel_spmd

F32 = mybir.dt.float32
BF16 = mybir.dt.bfloat16
I32 = mybir.dt.int32
AF = mybir.ActivationFunctionType
ALU = mybir.AluOpType
AX = mybir.AxisListType

S = 4096
D = 1024
H = 8
DH = 64
NG = 8
NE = 32
RB = 256
NBLK = (2 * S) // RB + NE
NROWS = NBLK * RB
NOFF = 11
BIG = 30000.0
SB_WINDOW = None
DEBUG = False
STOP_AFTER = 99


class Buf:
    def __init__(self, multi=False):
        self.ws = {}
        self.r = {}
        self.multi = multi


class Tl(Buf):
    def __init__(self, t):
        super().__init__()
        self.t = t


class Tracker:
    K = 12

    def __init__(self, nc, es):
        self.nc = nc
        self.eng = {}
        for name in ["tensor", "vector", "scalar", "gpsimd", "sync"]:
            sem = es.enter_context(nc.semaphore("c_" + name))
            self.eng[name] = dict(e=getattr(nc, name), sem=sem, n=0, seen={})
        self.dq = {}
        for q in ["sync", "gpsimd", "scalar"]:
            sems = [es.enter_context(nc.semaphore(f"d_{q}_{i}")) for i in range(self.K)]
            self.dq[q] = dict(sems=sems, cnt=[0] * self.K, toks=[None] * self.K, i=0)

    def wait(self, en, tok):
        if tok is None:
            return
        key, sem, val = tok
        E = self.eng[en]
        if E["seen"].get(key, 0) >= val:
            return
        if key == en and en in ("tensor", "sync"):
            return
        E["e"].wait_ge(sem, val)
        E["seen"][key] = val

    def _deps(self, reads, writes):
        deps = []
        for b in reads:
            deps.extend(b.ws.values())
        for b in writes:
            if not b.multi:
                deps.extend(b.ws.values())
            deps.extend(b.r.values())
        return deps

    def _mark(self, tok, reads, writes):
        for b in reads:
            b.r[tok[0]] = tok
        for b in writes:
            if b.multi:
                b.ws[tok[0]] = tok
            else:
                b.ws = {tok[0]: tok}
            b.r = {}

    def op(self, en, fn, reads=(), writes=()):
        for d in self._deps(reads, writes):
            self.wait(en, d)
        inst = fn()
        E = self.eng[en]
        E["n"] += 1
        inst.then_inc(E["sem"], 1)
        tok = (en, E["sem"], E["n"])
        E["seen"][en] = max(E["seen"].get(en, 0), 0)
        self._mark(tok, reads, writes)
        return tok

    def dma(self, q, out, in_, reads=(), writes=(), out_off=None, in_off=None, **ikw):
        Q = self.dq[q]
        i = Q["i"] % self.K
        Q["i"] += 1
        self.wait(q, Q["toks"][i])
        for d in self._deps(reads, writes):
            self.wait(q, d)
        e = self.eng[q]["e"]
        if out_off is None and in_off is None:
            inst = e.dma_start(out=out, in_=in_)
        else:
            inst = e.indirect_dma_start(out=out, out_offset=out_off, in_=in_, in_offset=in_off, **ikw)
        Q["cnt"][i] += 16
        inst.then_inc(Q["sems"][i], 16)
        tok = ((q, "dma", i), Q["sems"][i], Q["cnt"][i])
        Q["toks"][i] = tok
        self._mark(tok, reads, writes)
        return tok

    def barrier(self):
        toks = [(en, E["sem"], E["n"]) for en, E in self.eng.items() if E["n"] > 0]
        for Q in self.dq.values():
            toks += [t for t in Q["toks"] if t is not None]
        for en in self.eng:
            for t in toks:
                if t[0] == en and en in ("tensor", "sync"):
                    continue
                self.wait(en, t)


def rel_bucket_np(d):
    n = np.maximum(d, 0)
    me = 16
    nf = np.maximum(n, 1).astype(np.float32)
    large = me + (np.log(nf / np.float32(me)) / np.float32(math.log(1024 / me)) * np.float32(32 - me)).astype(np.int32)
    large = np.minimum(large, 31)
    return np.where(n < me, n, large)


def build_program():
    nc = bass.Bass("TRN2", target_bir_lowering=False)
    dbgk = "ExternalOutput" if DEBUG else "Internal"

    def din(name, shape, dt=F32):
        return nc.dram_tensor(name, list(shape), dt, kind="ExternalInput").ap()

    def dscr(name, shape, dt):
        return nc.dram_tensor(name, list(shape), dt, kind=dbgk).ap()

    x_d = din("x", [S, D])
    ccol_d = din("ccol", [128, 8])
    wada_d = din("w_ada", [D, 6 * D])
    bada_d = din("b_ada", [1, 6 * D])
    gmix_d = din("g_mix", [1, D])
    win_d = din("w_in", [D, 5120])
    gqk_d = din("gqk", [128, 2])
    wbra_d = din("w_br_moba", [512, D])
    wbrb_d = din("w_br_sb", [512, D])
    wout_d = din("w_out", [D, D])
    gffn_d = din("g_ffn", [1, D])
    wr_d = din("w_r", [D, 36])
    br_d = din("b_r", [1, 36])
    wg_d = din("w_gate_r", [NE * 128, 8 * 512])
    wu_d = din("w_up_r", [NE * 128, 8 * 512])
    wd_d = din("w_down_r", [NE * 128, 4 * D])
    thr_d = din("thr", [128, NE * 32])
    jv_d = din("jv", [128, NBLK * NE])
    pcol_d = din("pcol", [128, 1])
    identb_d = din("identb", [128, 128], BF16)
    identf_d = din("identf", [128, 128])
    blkdiag_d = din("blkdiag", [128, 128])
    emat_d = din("emat", [16, 16 * 128], BF16)
    negtri_d = din("negtri", [128, 128], BF16)
    negones_d = din("negones", [128, 128], BF16)
    masks_d = din("masks", [128, 4 * 512], BF16)
    ustrict_d = din("ustrict", [128, 128])
    onesf_d = din("onesf", [128, 128])
    baserow_d = din("baserow", [128, NE])
    biast_d = din("biast", [H, 128, NOFF * 512])
    bias31_d = din("bias31", [128, H])
    blkind_d = din("blkind", [16, S], BF16)
    diagm_d = din("diagm", [128, 128])
    antii_d = din("antii", [128, 128], BF16)

    out_d = nc.dram_tensor("out", [S, D], F32, kind="ExternalOutput").ap()

    mod_s = dscr("mod_s", [1, 6 * D], F32)
    qaT_s = dscr("qaT_s", [512, S], BF16)
    kaT_s = dscr("kaT_s", [512, S], BF16)
    qbT_s = dscr("qbT_s", [512, S], BF16)
    kbT_s = dscr("kbT_s", [512, S], BF16)
    va_s = dscr("va_s", [S, H * 65], BF16)
    vb_s = dscr("vb_s", [S, 512], BF16)
    gaT_s = dscr("gaT_s", [D, S], BF16)
    gbT_s = dscr("gbT_s", [D, S], BF16)
    mT_s = dscr("mT_s", [128, S], BF16)
    oaT_s = dscr("oaT_s", [512, S], BF16)
    obT_s = dscr("obT_s", [512, S], BF16)
    x1_s = dscr("x1_s", [S, D], F32)
    xg_s = dscr("xg_s", [NROWS, D], BF16)
    h2_s = dscr("h2_s", [S, D], BF16)
    dbgf_s = dscr("dbgf_s", [128, 1032], F32)
    dbgb_s = dscr("dbgb_s", [128, 256], BF16)
    y_s = dscr("y_s", [NROWS, D], F32)

    top = ExitStack()
    with top:
        T = Tracker(nc, top)
        build_program.T = T
        op, dma = T.op, T.dma

        def tile(es, name, shape, dt):
            return Tl(es.enter_context(nc.sbuf_tensor("sb_" + name, list(shape), dt)))

        def ptile(es, name, shape, dt):
            return Tl(es.enter_context(nc.psum_tensor("ps_" + name, list(shape), dt)))

        dB = {n: Buf(multi=True) for n in ["mod", "qaT", "kaT", "qbT", "kbT", "va", "vb", "gaT", "gbT", "mT", "oaT", "obT",
                                 "x1", "xg", "y", "out", "h2"]}

        A1 = tile(top, "A1", [128, D], F32)
        B1 = tile(top, "B1", [128, D], F32)
        gtA = tile(top, "gtA", [128, D], F32)
        A2 = tile(top, "A2", [128, D], F32)
        B2 = tile(top, "B2", [128, D], F32)
        gtF = tile(top, "gtF", [128, D], F32)
        identb = tile(top, "identb", [128, 128], BF16)
        identf = tile(top, "identf", [128, 128], F32)
        epsc = tile(top, "epsc", [128, 1], F32)
        onec = tile(top, "onec", [128, 1], F32)
        dA_all = tile(top, "dA_all", [128, 32], I32)
        dB_all = tile(top, "dB_all", [128, 32], I32)
        wA_all = tile(top, "wA_all", [128, 32], F32)
        wB_all = tile(top, "wB_all", [128, 32], F32)
        widx = tile(top, "widx", [128, NBLK], I32)
        dma("sync", identb.t[:], identb_d, writes=[identb])
        dma("sync", identf.t[:], identf_d, writes=[identf])
        op("vector", lambda: nc.vector.memset(epsc.t[:], 1e-6), writes=[epsc])
        op("vector", lambda: nc.vector.memset(onec.t[:], 1.0), writes=[onec])

        with ExitStack() as es:
            ccol = tile(es, "ccol", [128, 8], F32)
            silc = tile(es, "silc", [128, 8], F32)
            bada = tile(es, "bada", [1, 6 * D], F32)
            modrow = tile(es, "modrow", [1, 6 * D], F32)
            wa = [tile(es, f"wa{i}", [128, 8, 512], F32) for i in range(2)]
            pm = [ptile(es, f"pm{i}", [128, 512], F32) for i in range(2)]
            gm = tile(es, "gm", [128, D], F32)
            sc = tile(es, "sc", [128, D], F32)
            dma("sync", ccol.t[:], ccol_d, writes=[ccol])
            dma("sync", bada.t[:], bada_d, writes=[bada])
            op("scalar", lambda: nc.scalar.activation(out=silc.t[:], in_=ccol.t[:], func=AF.Silu),
               reads=[ccol], writes=[silc])
            wv = wada_d.rearrange("(kc p) c -> p kc c", p=128)
            for j in range(12):
                W = wa[j % 2]
                P = pm[j % 2]
                dma("sync", W.t[:], wv[:, :, j * 512:(j + 1) * 512], writes=[W])

                def mm():
                    for kc in range(8):
                        i = nc.tensor.matmul(P.t[0:1, :], lhsT=silc.t[:, kc:kc + 1], rhs=W.t[:, kc, :],
                                             start=(kc == 0), stop=(kc == 7))
                    return i
                op("tensor", mm, reads=[W, silc], writes=[P])
                op("vector", lambda: nc.vector.tensor_tensor(out=modrow.t[0:1, j * 512:(j + 1) * 512], in0=P.t[0:1, :],
                                                             in1=bada.t[0:1, j * 512:(j + 1) * 512], op=ALU.add),
                   reads=[P, bada], writes=[modrow])
            dma("sync", mod_s, modrow.t[:], reads=[modrow], writes=[dB["mod"]])

            def bc(dst, i):
                dma("sync", dst.t[:], mod_s[0:1, i * D:(i + 1) * D].partition_broadcast(128), reads=[dB["mod"]],
                    writes=[dst])
            bc(B1, 0)
            bc(gtA, 2)
            bc(B2, 3)
            bc(gtF, 5)
            dma("sync", gm.t[:], gmix_d[0:1, :].partition_broadcast(128), writes=[gm])
            bc(sc, 1)
            op("vector", lambda: nc.vector.scalar_tensor_tensor(out=A1.t[:], in0=sc.t[:], scalar=1.0, in1=gm.t[:],
                                                                op0=ALU.add, op1=ALU.mult),
               reads=[sc, gm], writes=[A1])
            dma("sync", gm.t[:], gffn_d[0:1, :].partition_broadcast(128), writes=[gm])
            bc(sc, 4)
            op("vector", lambda: nc.vector.scalar_tensor_tensor(out=A2.t[:], in0=sc.t[:], scalar=1.0, in1=gm.t[:],
                                                                op0=ALU.add, op1=ALU.mult),
               reads=[sc, gm], writes=[A2])
            T.barrier()
        if STOP_AFTER <= 0:
            T.barrier()
            return nc

        def norm_mod(xt, A, B, hf, hb, junk, ss, std, rstd):
            op("scalar", lambda: nc.scalar.activation(out=junk.t[:], in_=xt.t[:], func=AF.Square,
                                                      accum_out=ss.t[:, 0:1]), reads=[xt], writes=[junk, ss])
            op("scalar", lambda: nc.scalar.activation(out=std.t[:], in_=ss.t[:], func=AF.Sqrt, scale=1.0 / D,
                                                      bias=epsc.t[:, 0:1]), reads=[ss, epsc], writes=[std])
            op("vector", lambda: nc.vector.reciprocal(out=rstd.t[:], in_=std.t[:]), reads=[std], writes=[rstd])
            op("vector", lambda: nc.vector.scalar_tensor_tensor(out=hf.t[:], in0=xt.t[:], scalar=rstd.t[:, 0:1],
                                                                in1=A.t[:], op0=ALU.mult, op1=ALU.mult),
               reads=[xt, rstd, A], writes=[hf])
            if hb is not None:
                op("gpsimd", lambda: nc.gpsimd.tensor_tensor(out=hb.t[:], in0=hf.t[:], in1=B.t[:], op=ALU.add),
                   reads=[hf, B], writes=[hb])

        with ExitStack() as es:
            win = tile(es, "win", [128, 8, 5120], BF16)
            winv = win_d.rearrange("(kc p) c -> p kc c", p=128)
            for kc in range(8):
                dma("gpsimd", win.t[:, kc, :], winv[:, kc, :], writes=[win])
            gqk = tile(es, "gqk", [128, 2], F32)
            gqs = tile(es, "gqs", [128, 1], F32)
            blkdiag = tile(es, "blkdiag", [128, 128], F32)
            dma("sync", gqk.t[:], gqk_d, writes=[gqk])
            dma("sync", blkdiag.t[:], blkdiag_d, writes=[blkdiag])
            op("vector", lambda: nc.vector.tensor_scalar(out=gqs.t[:], in0=gqk.t[:, 0:1], scalar1=0.125, scalar2=None,
                                                         op0=ALU.mult), reads=[gqk], writes=[gqs])
            kmean = tile(es, "kmean", [128, 4, 16], F32)
            op("vector", lambda: nc.vector.memset(kmean.t[:], 0.0), writes=[kmean])
            xb_ = [tile(es, f"xt{i}", [128, D], F32) for i in range(2)]
            hf = tile(es, "hf", [128, D], F32)
            hb = [tile(es, f"hb{i}", [128, D], BF16) for i in range(2)]
            junk = tile(es, "junk", [128, D], BF16)
            ss = tile(es, "ss", [128, 1], F32)
            std = tile(es, "std", [128, 1], F32)
            rstd = tile(es, "rstd", [128, 1], F32)
            hT = [tile(es, f"hT{i}", [128, 8, 512], BF16) for i in range(2)]
            vat = [tile(es, f"vat{i}", [128, H, 65], BF16) for i in range(2)]
            vbt = [tile(es, f"vbt{i}", [128, 512], BF16) for i in range(2)]
            for v in vat:
                op("vector", lambda: nc.vector.memset(v.t[:], 1.0), writes=[v])
            sq = tile(es, "sq", [128, 512], F32)
            sq_ = [sq, tile(es, "sq1", [128, 512], F32)]
            stdt = tile(es, "stdt", [128, 512], F32)
            rstdt = tile(es, "rstdt", [128, 512], F32)
            qn = tile(es, "qn", [128, 512], F32)
            ob_ = [tile(es, f"ob{i}", [128, 512], BF16) for i in range(3)]
            km2 = tile(es, "km2", [128, 2], F32)
            gsb = tile(es, "gsb", [128, 4, H, 16], F32)
            top8 = tile(es, "top8", [128, 4, H, 8], F32)
            sel = tile(es, "sel", [128, 4, H, 16], F32)
            mtb = tile(es, "mtb", [128, 512], BF16)
            ptr = [ptile(es, f"ptr{i}", [128, 1024], BF16) for i in range(2)]
            pmm = [ptile(es, f"pmm{i}", [128, 512], F32) for i in range(3)]
            pms = ptile(es, "pms", [128, 512], F32)
            pg = ptile(es, "pg", [128, 512], F32)
            pmt = ptile(es, "pmt", [128, 512], F32)
            obi = [0]
            pmi = [0]

            def next_ob():
                obi[0] += 1
                return ob_[obi[0] % 3]

            def next_pmm():
                pmi[0] += 1
                return pmm[pmi[0] % 3]

            def fm_chunk(hTg, c0):
                P = next_pmm()

                def mm():
                    for kc in range(8):
                        i = nc.tensor.matmul(P.t[:], lhsT=win.t[:, kc, c0:c0 + 128], rhs=hTg.t[:, kc, :],
                                             start=(kc == 0), stop=(kc == 7))
                    return i
                op("tensor", mm, reads=[win, hTg], writes=[P])
                return P

            def qknorm(P, gcol):
                op("scalar", lambda: nc.scalar.activation(out=sq.t[:], in_=P.t[:], func=AF.Square), reads=[P], writes=[sq])
                op("tensor", lambda: nc.tensor.matmul(pms.t[:], lhsT=blkdiag.t[:], rhs=sq.t[:], start=True, stop=True),
                   reads=[blkdiag, sq], writes=[pms])
                op("scalar", lambda: nc.scalar.activation(out=stdt.t[:], in_=pms.t[:], func=AF.Sqrt, bias=epsc.t[:, 0:1]),
                   reads=[pms, epsc], writes=[stdt])
                op("vector", lambda: nc.vector.reciprocal(out=rstdt.t[:], in_=stdt.t[:]), reads=[stdt], writes=[rstdt])
                op("vector", lambda: nc.vector.scalar_tensor_tensor(out=qn.t[:], in0=P.t[:], scalar=gcol, in1=rstdt.t[:],
                                                                    op0=ALU.mult, op1=ALU.mult),
                   reads=[P, rstdt, gqk, gqs], writes=[qn])

            def front_a(g, tt):
                k = g * 4 + tt
                tok0 = k * 128
                xt = xb_[k % 2]
                hbk = hb[k % 2]
                dma("sync", xt.t[:], x_d[tok0:tok0 + 128, :], writes=[xt])
                norm_mod(xt, A1, B1, hf, hbk, junk, ss, std, rstd)

            def front_b(g, tt):
                hTg = hT[g % 2]
                k = g * 4 + tt
                hbk = hb[k % 2]
                PT = ptr[k % 2]

                def tr():
                    for kc in range(8):
                        i = nc.tensor.transpose(out=PT.t[:, kc * 128:(kc + 1) * 128],
                                                in_=hbk.t[:, kc * 128:(kc + 1) * 128], identity=identb.t[:])
                    return i
                op("tensor", tr, reads=[hbk, identb], writes=[PT])
                op("vector", lambda: nc.vector.tensor_copy(out=hTg.t[:, :, tt * 128:(tt + 1) * 128],
                                                           in_=PT.t[:].rearrange("p (kc t) -> p kc t", kc=8)),
                   reads=[PT], writes=[hTg])

            def back(g):
                hTg = hT[g % 2]
                for tt in range(4):
                    k = g * 4 + tt
                    tok0 = k * 128
                    for (c0, isA) in ((1024, True), (2560, False)):
                        P = next_pmm()

                        def mm():
                            for kc in range(8):
                                i = nc.tensor.matmul(P.t[:], lhsT=hTg.t[:, kc, tt * 128:(tt + 1) * 128],
                                                     rhs=win.t[:, kc, c0:c0 + 512], start=(kc == 0), stop=(kc == 7))
                            return i
                        op("tensor", mm, reads=[win, hTg], writes=[P])
                        if isA:
                            V = vat[k % 2]
                            op("vector", lambda: nc.vector.tensor_copy(out=V.t[:, :, 0:64],
                                                                       in_=P.t[:].rearrange("p (h d) -> p h d", h=H)),
                               reads=[P], writes=[V])
                            dma("gpsimd", va_s[tok0:tok0 + 128, :], V.t[:].rearrange("p h d -> p (h d)"), reads=[V],
                                writes=[dB["va"]])
                        else:
                            V = vbt[k % 2]
                            op("scalar", lambda: nc.scalar.activation(out=V.t[:], in_=P.t[:], func=AF.Copy), reads=[P],
                               writes=[V])
                            dma("gpsimd", vb_s[tok0:tok0 + 128, :], V.t[:], reads=[V], writes=[dB["vb"]])
                    yield
                chunks = [("k", j) for j in range(4)] + [("q", j) for j in range(4)]
                held = {}

                def stA(i):
                    kind, j = chunks[i]
                    P = fm_chunk(hTg, (512 if kind == "k" else 0) + j * 128)
                    SQ = sq_[i % 2]
                    op("scalar", lambda: nc.scalar.activation(out=SQ.t[:], in_=P.t[:], func=AF.Square), reads=[P], writes=[SQ])
                    held[i] = (P, SQ)

                def stB(i):
                    kind, j = chunks[i]
                    P, SQ = held.pop(i)
                    gcol = gqk.t[:, 1:2] if kind == "k" else gqs.t[:, 0:1]
                    op("tensor", lambda: nc.tensor.matmul(pms.t[:], lhsT=blkdiag.t[:], rhs=SQ.t[:], start=True, stop=True),
                       reads=[blkdiag, SQ], writes=[pms])
                    op("scalar", lambda: nc.scalar.activation(out=stdt.t[:], in_=pms.t[:], func=AF.Sqrt, bias=epsc.t[:, 0:1]),
                       reads=[pms, epsc], writes=[stdt])
                    op("vector", lambda: nc.vector.reciprocal(out=rstdt.t[:], in_=stdt.t[:]), reads=[stdt], writes=[rstdt])
                    op("vector", lambda: nc.vector.scalar_tensor_tensor(out=qn.t[:], in0=P.t[:], scalar=gcol, in1=rstdt.t[:],
                                                                        op0=ALU.mult, op1=ALU.mult),
                       reads=[P, rstdt, gqk, gqs], writes=[qn])
                    if kind == "k":
                        op("vector", lambda: nc.vector.tensor_reduce(out=km2.t[:], in_=qn.t[:].rearrange("p (b t) -> p b t", b=2),
                                                                     axis=AX.X, op=ALU.add), reads=[qn], writes=[km2])
                        op("vector", lambda: nc.vector.tensor_scalar(out=kmean.t[:, j, 2 * g:2 * g + 2], in0=km2.t[:],
                                                                     scalar1=1.0 / 256, scalar2=None, op0=ALU.mult),
                           reads=[km2], writes=[kmean])
                        dst, nm = kaT_s, "kaT"
                    else:
                        def gm_():
                            for hh in range(2):
                                for tt in range(4):
                                    h = 2 * j + hh
                                    i_ = nc.tensor.matmul(pg.t[:, (tt * H + h) * 16:(tt * H + h + 1) * 16],
                                                          lhsT=qn.t[hh * 64:(hh + 1) * 64, tt * 128:(tt + 1) * 128],
                                                          rhs=kmean.t[hh * 64:(hh + 1) * 64, j, :], start=True, stop=True)
                            return i_
                        op("tensor", gm_, reads=[qn, kmean], writes=[pg])
                        dst, nm = qaT_s, "qaT"
                    O = next_ob()
                    op("gpsimd", lambda: nc.gpsimd.tensor_copy(out=O.t[:], in_=qn.t[:]), reads=[qn], writes=[O])
                    dma("scalar", dst[j * 128:(j + 1) * 128, g * 512:(g + 1) * 512], O.t[:], reads=[O], writes=[dB[nm]])

                stA(0)
                for i in range(8):
                    if i + 1 < 8:
                        stA(i + 1)
                    stB(i)
                    yield
                op("scalar", lambda: nc.scalar.activation(out=gsb.t[:].rearrange("p a b c -> p (a b c)"), in_=pg.t[:],
                                                          func=AF.Copy), reads=[pg], writes=[gsb])
                for tt in range(4):
                    own = 2 * g + tt // 2
                    op("vector", lambda: nc.vector.memset(gsb.t[:, tt, :, own:16], -1e30), writes=[gsb])
                for tt in range(4):
                    for h in range(H):
                        op("vector", lambda: nc.vector.max(out=top8.t[:, tt, h, :], in_=gsb.t[:, tt, h, :]), reads=[gsb],
                           writes=[top8])
                op("vector", lambda: nc.vector.tensor_tensor(out=sel.t[:], in0=gsb.t[:],
                                                             in1=top8.t[:, :, :, 2:3].to_broadcast([128, 4, H, 16]),
                                                             op=ALU.is_ge), reads=[gsb, top8], writes=[sel])
                for tt in range(4):
                    own = 2 * g + tt // 2
                    op("vector", lambda: nc.vector.memset(sel.t[:, tt, :, own:own + 1], 1.0), writes=[sel])
                    if own + 1 < 16:
                        op("vector", lambda: nc.vector.memset(sel.t[:, tt, :, own + 1:16], 0.0), writes=[sel])
                op("vector", lambda: nc.vector.tensor_scalar(out=sel.t[:], in0=sel.t[:], scalar1=1.0, scalar2=BIG,
                                                             op0=ALU.subtract, op1=ALU.mult), reads=[sel], writes=[sel])

                def trm():
                    for tt in range(4):
                        i = nc.tensor.transpose(out=pmt.t[:, tt * 128:(tt + 1) * 128],
                                                in_=sel.t[:, tt, :, :].rearrange("p a b -> p (a b)"), identity=identf.t[:])
                    return i
                op("tensor", trm, reads=[sel, identf], writes=[pmt])
                op("scalar", lambda: nc.scalar.activation(out=mtb.t[:], in_=pmt.t[:], func=AF.Copy), reads=[pmt], writes=[mtb])
                dma("scalar", mT_s[:, g * 512:(g + 1) * 512], mtb.t[:], reads=[mtb], writes=[dB["mT"]])
                yield
                for j in range(4):
                    P = fm_chunk(hTg, 1536 + j * 128)
                    O = next_ob()
                    op("scalar", lambda: nc.scalar.activation(out=O.t[:], in_=P.t[:], func=AF.Copy, scale=0.125),
                       reads=[P], writes=[O])
                    dma("scalar", qbT_s[j * 128:(j + 1) * 128, g * 512:(g + 1) * 512], O.t[:], reads=[O], writes=[dB["qbT"]])
                    yield
                for j in range(4):
                    P = fm_chunk(hTg, 2048 + j * 128)
                    O = next_ob()
                    op("vector", lambda: nc.vector.tensor_copy(out=O.t[:], in_=P.t[:]), reads=[P], writes=[O])
                    dma("scalar", kbT_s[j * 128:(j + 1) * 128, g * 512:(g + 1) * 512], O.t[:], reads=[O], writes=[dB["kbT"]])
                    yield
                for (c0, dst, nm) in ((3072, gaT_s, "gaT"), (4096, gbT_s, "gbT")):
                    for j in range(8):
                        P = fm_chunk(hTg, c0 + j * 128)
                        O = next_ob()
                        op("scalar", lambda: nc.scalar.activation(out=O.t[:], in_=P.t[:], func=AF.Sigmoid), reads=[P],
                           writes=[O])
                        dma("scalar", dst[j * 128:(j + 1) * 128, g * 512:(g + 1) * 512], O.t[:], reads=[O], writes=[dB[nm]])
                        yield
            for tt in range(4):
                front_a(0, tt)
                front_b(0, tt)
            PA = {3: 0, 11: 1, 19: 2, 27: 3}
            PB = {8: 0, 16: 1, 24: 2, 32: 3}
            for g in range(NG):
                n_y = 0
                for _ in back(g):
                    n_y += 1
                    if g + 1 < NG:
                        if n_y in PA:
                            front_a(g + 1, PA[n_y])
                        if n_y in PB:
                            front_b(g + 1, PB[n_y])
                assert n_y >= 32, n_y
            T.barrier()
        if STOP_AFTER <= 1:
            T.barrier()
            return nc

        with ExitStack() as es:
            vaug = tile(es, "vaug", [128, 32, H * 65], BF16)
            dma("gpsimd", vaug.t[:], va_s.rearrange("(kb p) c -> p kb c", p=128), reads=[dB["va"]], writes=[vaug])
            zt = tile(es, "zt", [128, 8 * D], BF16)
            op("gpsimd", lambda: nc.gpsimd.memset(zt.t[:], 0.0), writes=[zt])
            xgv = xg_s.rearrange("(p r) d -> p r d", p=128)
            for zi in range(NROWS // 128 // 8):
                dma("gpsimd", xgv[:, zi * 8:(zi + 1) * 8, :], zt.t[:].rearrange("p (r d) -> p r d", r=8), reads=[zt],
                    writes=[dB["xg"]])
            b31 = tile(es, "b31", [128, H], F32)
            dma("sync", b31.t[:], bias31_d, writes=[b31])
            onesf = tile(es, "onesf", [128, 128], F32)
            dma("sync", onesf.t[:], onesf_d, writes=[onesf])
            qT = [tile(es, f"qT{i}", [80, S], BF16) for i in range(2)]
            kT = [tile(es, f"kT{i}", [80, S], BF16) for i in range(2)]
            for kt_ in kT:
                dma("sync", kt_.t[64:80, :], blkind_d, writes=[kt_])
            bt = [tile(es, f"bt{i}", [128, NOFF * 512], F32) for i in range(2)]
            NTMP, NPB, NPS = 3, 5, 4
            tmp = [tile(es, f"tmp{i}", [128, 512], F32) for i in range(NTMP)]
            pb = [tile(es, f"pb{i}", [128, 512], BF16) for i in range(NPB)]
            osb = [tile(es, f"osb{i}", [64, 512], F32) for i in range(2)]
            rden = [tile(es, f"rden{i}", [128, 512], F32) for i in range(2)]
            onb = [tile(es, f"onb{i}", [64, 512], BF16) for i in range(2)]
            pS = [ptile(es, f"pS{i}", [128, 512], F32) for i in range(NPS)]
            pacc = [ptile(es, f"pacc{i}", [128, 512], F32) for i in range(2)]
            pbc = ptile(es, "pbc", [128, 512], F32)

            def load_head(h):
                i = h % 2
                dma("sync", qT[i].t[0:64, :], qaT_s[h * 64:(h + 1) * 64, :], reads=[dB["qaT"]], writes=[qT[i]])
                dma("sync", qT[i].t[64:80, :], mT_s[h * 16:(h + 1) * 16, :], reads=[dB["mT"]], writes=[qT[i]])
                dma("sync", kT[i].t[0:64, :], kaT_s[h * 64:(h + 1) * 64, :], reads=[dB["kaT"]], writes=[kT[i]])
                dma("sync", bt[i].t[:], biast_d[h], writes=[bt[i]])
            jobs = [(h, qt, kb) for h in range(H) for qt in range(8) for kb in range(4 * qt + 4)]
            NJ = len(jobs)
            LAG = 3
            load_head(0)
            fin2 = {}

            def stage_front(t):
                h, qt, kb = jobs[t]
                if qt == 0 and kb == 0 and h + 1 < H:
                    load_head(h + 1)
                Q, K_, BT = qT[h % 2], kT[h % 2], bt[h % 2]
                Sx = pS[t % NPS]
                op("tensor", lambda: nc.tensor.matmul(Sx.t[:], lhsT=K_.t[:, kb * 128:(kb + 1) * 128],
                                                      rhs=Q.t[:, qt * 512:(qt + 1) * 512], start=True, stop=True),
                   reads=[K_, Q], writes=[Sx])
                delta = qt * 512 - kb * 128
                P = pb[t % NPB]
                if delta < 917:
                    oi = (delta + 384) // 128
                    tm = tmp[t % NTMP]
                    op("vector", lambda: nc.vector.tensor_tensor(out=tm.t[:], in0=Sx.t[:],
                                                                 in1=BT.t[:, oi * 512:(oi + 1) * 512], op=ALU.add),
                       reads=[Sx, BT], writes=[tm])
                    op("scalar", lambda: nc.scalar.activation(out=P.t[:], in_=tm.t[:], func=AF.Exp), reads=[tm], writes=[P])
                else:
                    op("scalar", lambda: nc.scalar.activation(out=P.t[:], in_=Sx.t[:], func=AF.Exp, bias=b31.t[:, h:h + 1]),
                       reads=[Sx, b31], writes=[P])

            def stage_back(t, step):
                h, qt, kb = jobs[t]
                nkb = 4 * qt + 4
                qi = h * 8 + qt
                ACC = pacc[qi % 2]
                P = pb[t % NPB]
                op("tensor", lambda: nc.tensor.matmul(ACC.t[0:65, :], lhsT=vaug.t[:, kb, h * 65:(h + 1) * 65], rhs=P.t[:],
                                                      start=(kb == 0), stop=(kb == nkb - 1)), reads=[vaug, P], writes=[ACC])
                if kb == nkb - 1:
                    OS, RD = osb[qi % 2], rden[qi % 2]
                    op("scalar", lambda: nc.scalar.activation(out=OS.t[:], in_=ACC.t[0:64, :], func=AF.Copy), reads=[ACC],
                       writes=[OS])
                    op("vector", lambda: nc.vector.reciprocal(out=RD.t[64:65, :], in_=ACC.t[64:65, :]), reads=[ACC], writes=[RD])

                    def f2():
                        op("tensor", lambda: nc.tensor.matmul(pbc.t[0:64, :], lhsT=onesf.t[64:65, 0:64], rhs=RD.t[64:65, :],
                                                              start=True, stop=True), reads=[onesf, RD], writes=[pbc])
                        ON = onb[qi % 2]
                        op("vector", lambda: nc.vector.tensor_tensor(out=ON.t[:], in0=OS.t[:], in1=pbc.t[0:64, :], op=ALU.mult),
                           reads=[OS, pbc], writes=[ON])
                        dma("gpsimd", oaT_s[h * 64:(h + 1) * 64, qt * 512:(qt + 1) * 512], ON.t[:], reads=[ON],
                            writes=[dB["oaT"]])
                    fin2.setdefault(step + 3, []).append(f2)

            for step in range(NJ + LAG + 5):
                if step < NJ:
                    stage_front(step)
                if 0 <= step - LAG < NJ:
                    stage_back(step - LAG, step)
                for f in fin2.pop(step, []):
                    f()
            assert not fin2
            T.barrier()
        if STOP_AFTER <= 2:
            T.barrier()
            return nc

        with ExitStack() as es:
            dvh = tile(es, "dvh", [128, 32, 512], BF16)
            dvl = tile(es, "dvl", [128, 32, 512], BF16)
            vbv = vb_s.rearrange("(kb p) c -> p kb c", p=128)
            antiI = tile(es, "antiI", [128, 128], BF16)
            nantiI = tile(es, "nantiI", [128, 128], BF16)
            dma("sync", antiI.t[:], antii_d, writes=[antiI])
            op("vector", lambda: nc.vector.tensor_scalar(out=nantiI.t[:], in0=antiI.t[:], scalar1=-1.0, scalar2=None, op0=ALU.mult),
               reads=[antiI], writes=[nantiI])
            NOM, NPBUF, NXT, NZ, NPT = 3, 4, 4, 3, 2
            pz = [ptile(es, f"pz{i}", [128, 512], F32) for i in range(NZ)]
            ptt = [ptile(es, f"ptt{i}", [128, 512], F32) for i in range(NPT)]
            pacc = [ptile(es, f"spacc{i}", [128, 512], F32) for i in range(2)]
            ptot = ptile(es, "ptot", [128, 512], F32)
            with ExitStack() as es_in:
                vb = tile(es_in, "vb", [128, 32, 512], BF16)
                vb1 = tile(es_in, "vb1", [128, 32, 512], BF16)
                dma("gpsimd", vb.t[:], vbv, reads=[dB["vb"]], writes=[vb])
                op("gpsimd", lambda: nc.gpsimd.memset(vb1.t[:], 0.0), writes=[vb1])
                dma("sync", vb1.t[:, 0:31, :], vb_s[1:1 + 31 * 128, :].rearrange("(kb p) c -> p kb c", p=128), reads=[dB["vb"]],
                    writes=[vb1])
                dma("sync", vb1.t[0:127, 31, :], vb_s[31 * 128 + 1:S, :], reads=[dB["vb"]], writes=[vb1])
                for kb in range(32):
                    Pz = pz[kb % NZ]

                    def mmr():
                        nc.tensor.matmul(Pz.t[:], lhsT=antiI.t[:], rhs=vb.t[:, kb, :], start=True, stop=False)
                        return nc.tensor.matmul(Pz.t[:], lhsT=nantiI.t[:], rhs=vb1.t[:, kb, :], start=False, stop=True)
                    op("tensor", mmr, reads=[antiI, nantiI, vb, vb1], writes=[Pz])
                    op("scalar", lambda: nc.scalar.activation(out=dvh.t[:, kb, :], in_=Pz.t[:], func=AF.Copy), reads=[Pz],
                       writes=[dvh])
                    op("vector", lambda: nc.vector.tensor_tensor(out=dvl.t[:, kb, :], in0=Pz.t[:], in1=dvh.t[:, kb, :],
                                                                 op=ALU.subtract), reads=[Pz, dvh], writes=[dvl])
                T.barrier()
            diagm = tile(es, "diagm", [128, 128], F32)
            dma("sync", diagm.t[:], diagm_d, writes=[diagm])
            zer = tile(es, "zer", [128, 512], F32)
            op("vector", lambda: nc.vector.memset(zer.t[:], 0.0), writes=[zer])
            onesrow = tile(es, "onesrow", [1, 128], BF16)
            op("vector", lambda: nc.vector.memset(onesrow.t[:], 1.0), writes=[onesrow])
            qT = [tile(es, f"sqT{i}", [64, S], BF16) for i in range(2)]
            kT = [tile(es, f"skT{i}", [64, S], BF16) for i in range(2)]
            vfirst_ = [tile(es, f"vfirst{i}", [1, 32, 64], BF16) for i in range(3)]
            oms = [tile(es, f"oms{i}", [128, 516], F32) for i in range(NOM)]
            pbx = [tile(es, f"pbx{i}", [128, 512], F32) for i in range(NPBUF)]
            pxt = [tile(es, f"pxt{i}", [128, 512], BF16) for i in range(NXT)]
            tot = [tile(es, f"tot{i}", [128, 1], F32) for i in range(2)]
            dgt = [tile(es, f"dgt{i}", [128, 128], BF16) for i in range(8)]
            oacc = [tile(es, f"oacc{i}", [64, 128], F32) for i in range(2)]
            zcol = tile(es, "zcol", [128, 128], BF16)
            op("vector", lambda: nc.vector.memset(zcol.t[:], 0.0), writes=[zcol])
            v0b_ = [tile(es, f"v0b{i}", [128, 64], BF16) for i in range(3)]
            onb = [tile(es, f"sonb{i}", [64, 512], BF16) for i in range(2)]

            def load_head(h):
                i = h % 2
                dma("sync", qT[i].t[:], qbT_s[h * 64:(h + 1) * 64, :], reads=[dB["qbT"]], writes=[qT[i]])
                dma("sync", kT[i].t[:], kbT_s[h * 64:(h + 1) * 64, :], reads=[dB["kbT"]], writes=[kT[i]])
                i3 = h % 3
                dma("sync", vfirst_[i3].t[:], vbv[0:1, :, h * 64:(h + 1) * 64], reads=[dB["vb"]], writes=[vfirst_[i3]])
                dma("sync", v0b_[i3].t[:], vb_s[0:1, h * 64:(h + 1) * 64].partition_broadcast(128), reads=[dB["vb"]],
                    writes=[v0b_[i3]])
                op("vector", lambda: nc.vector.tensor_scalar(out=v0b_[i3].t[:], in0=v0b_[i3].t[:], scalar1=-1.0, scalar2=None,
                                                             op0=ALU.mult), reads=[v0b_[i3]], writes=[v0b_[i3]])
            jobs = []
            for h in range(H):
                for qi in range(32):
                    q0 = qi * 128
                    nch = -(-(q0 + 128) // 512)
                    for c in range(nch):
                        k_hi = q0 + 128 - 512 * c
                        k_lo = max(0, k_hi - 512)
                        jobs.append((h, qi, c, nch, k_lo, k_hi))
            NJ = len(jobs)
            load_head(0)

            def stF(t):
                h, qi, c, nch, k_lo, k_hi = jobs[t]
                if qi == 0 and c == 0 and h + 1 < H:
                    load_head(h + 1)
                w = k_hi - k_lo
                q0 = qi * 128
                Q, K_ = qT[h % 2], kT[h % 2]
                Z, OMS, PBX = pz[t % NZ], oms[t % NOM], pbx[t % NPBUF]
                op("tensor", lambda: nc.tensor.matmul(Z.t[:, 0:w], lhsT=Q.t[:, q0:q0 + 128], rhs=K_.t[:, k_lo:k_hi],
                                                      start=True, stop=True), reads=[Q, K_], writes=[Z])
                op("scalar", lambda: nc.scalar.activation(out=OMS.t[:, w:0:-1], in_=Z.t[:, 0:w], func=AF.Sigmoid, scale=-1.0),
                   reads=[Z], writes=[OMS])
                if c < nch - 1:
                    OMSn = oms[(t + 1) % NOM]
                    op("scalar", lambda: nc.scalar.activation(out=OMSn.t[:, 0:1], in_=Z.t[:, 0:1], func=AF.Sigmoid, scale=-1.0),
                       reads=[Z], writes=[OMSn])
                if c == 0:
                    op("vector", lambda: nc.vector.tensor_tensor(out=OMS.t[:, 1:129], in0=OMS.t[:, 1:129], in1=diagm.t[:],
                                                                 op=ALU.max), reads=[OMS, diagm], writes=[OMS])
                    op("vector", lambda: nc.vector.memset(OMS.t[:, 0:1], 1.0), writes=[OMS])
                    init = 1.0
                    rd = [OMS, zer]
                else:
                    PBp = pbx[(t - 1) % NPBUF]
                    wp = jobs[t - 1][5] - jobs[t - 1][4]
                    init = PBp.t[:, wp - 1:wp]
                    rd = [OMS, zer, PBp]
                op("vector", lambda: nc.vector.tensor_tensor_scan(out=PBX.t[:, 0:w], data0=OMS.t[:, 0:w], data1=zer.t[:, 0:w],
                                                                  initial=init, op0=ALU.mult, op1=ALU.add),
                   reads=rd, writes=[PBX])
                if c == nch - 1:
                    TT = tot[(h * 32 + qi) % 2]
                    op("vector", lambda: nc.vector.tensor_tensor(out=TT.t[:], in0=PBX.t[:, w - 1:w], in1=OMS.t[:, w:w + 1],
                                                                 op=ALU.mult), reads=[PBX, OMS], writes=[TT])
                    DG = dgt[(h * 32 + qi) % 8]
                    op("vector", lambda: nc.vector.tensor_scalar(out=DG.t[:], in0=identf.t[:], scalar1=TT.t[:, 0:1], scalar2=None,
                                                                 op0=ALU.mult), reads=[identf, TT], writes=[DG])
                    if DEBUG and t == 0:
                        dma("sync", dbgf_s[:, 0:2], OMS.t[:, 127:129], reads=[OMS])
                        tt2 = tile(es, "tt2", [128, 2], F32)
                        op("vector", lambda: nc.vector.tensor_copy(out=tt2.t[:], in_=TT.t[:, 0:1].to_broadcast([128, 2])), reads=[TT], writes=[tt2])
                        dma("sync", dbgf_s[:, 2:4], tt2.t[:], reads=[tt2])
                        dma("sync", dbgf_s[:, 1:517], OMS.t[:, 0:516], reads=[OMS])
                        dma("sync", dbgf_s[:, 520:1032], PBX.t[:, :], reads=[PBX])
                        dma("sync", dbgb_s[:, 0:128], DG.t[:], reads=[DG])
                        dma("sync", dbgb_s[:, 128:192], v0b_[0].t[:], reads=[v0b_[0]])

            def stT(t):
                h, qi, c, nch, k_lo, k_hi = jobs[t]
                w = k_hi - k_lo
                PBX, PT, PXT = pbx[t % NPBUF], ptt[t % NPT], pxt[t % NXT]

                def tr():
                    for b in range(w // 128):
                        i = nc.tensor.transpose(out=PT.t[:, b * 128:(b + 1) * 128], in_=PBX.t[:, b * 128:(b + 1) * 128],
                                                identity=identf.t[:])
                    return i
                op("tensor", tr, reads=[PBX, identf], writes=[PT])
                op("scalar", lambda: nc.scalar.activation(out=PXT.t[:, 0:w], in_=PT.t[:, 0:w], func=AF.Copy), reads=[PT],
                   writes=[PXT])

            def stP(t):
                h, qi, c, nch, k_lo, k_hi = jobs[t]
                w = k_hi - k_lo
                PXT, ACC = pxt[t % NXT], pacc[(h * 32 + qi) % 2]
                nb = w // 128
                hs = slice(h * 64, (h + 1) * 64)
                vfirst = vfirst_[h % 3]
                last = (c == nch - 1)

                def mm():
                    for b in range(nb):
                        kb = (k_hi - 128 * (b + 1)) // 128
                        nc.tensor.matmul(ACC.t[0:64, 0:128], lhsT=dvh.t[:, kb, hs], rhs=PXT.t[:, b * 128:(b + 1) * 128],
                                         start=(c == 0 and b == 0), stop=False)
                        i = nc.tensor.matmul(ACC.t[0:64, 0:128], lhsT=dvl.t[:, kb, hs], rhs=PXT.t[:, b * 128:(b + 1) * 128],
                                             start=False, stop=False)
                    return i
                op("tensor", mm, reads=[dvh, dvl, PXT], writes=[ACC])
                if last:
                    DG, V0B = dgt[(h * 32 + qi) % 8], v0b_[h % 3]

                    def mm2():
                        if qi < 31:
                            return nc.tensor.matmul(ACC.t[0:64, 0:128], lhsT=vfirst.t[0:1, qi + 1, :], rhs=onesrow.t[0:1, :],
                                                    start=False, stop=True)
                        return nc.tensor.matmul(ACC.t[0:64, 0:128], lhsT=dvl.t[:, 0, hs], rhs=zcol.t[:, :], start=False, stop=True)
                    op("tensor", mm2, reads=[vfirst, onesrow, zcol, dvl], writes=[ACC])
                    op("tensor", lambda: nc.tensor.matmul(ptot.t[0:64, 0:128], lhsT=V0B.t[:, :], rhs=DG.t[:, :], start=True, stop=True),
                       reads=[DG, V0B], writes=[ptot])
                    OA = oacc[(h * 32 + qi) % 2]
                    op("scalar", lambda: nc.scalar.activation(out=OA.t[:], in_=ACC.t[0:64, 0:128], func=AF.Copy), reads=[ACC], writes=[OA])
                    ON = onb[((h * 32 + qi) // 4) % 2]
                    op("vector", lambda: nc.vector.tensor_tensor(out=ON.t[:, (qi % 4) * 128:(qi % 4 + 1) * 128], in0=OA.t[:],
                                                                 in1=ptot.t[0:64, 0:128], op=ALU.add), reads=[OA, ptot], writes=[ON])
                    if qi % 4 == 3:
                        g4 = qi // 4
                        dma("gpsimd", obT_s[h * 64:(h + 1) * 64, g4 * 512:(g4 + 1) * 512], ON.t[:], reads=[ON], writes=[dB["obT"]])

            L1, L2 = 3, 5
            for step in range(NJ + L2 + 1):
                if step < NJ:
                    stF(step)
                if 0 <= step - L1 < NJ:
                    stT(step - L1)
                if 0 <= step - L2 < NJ:
                    stP(step - L2)
            T.barrier()
        if STOP_AFTER <= 3:
            T.barrier()
            return nc

        with ExitStack() as es:
            wbra = tile(es, "wbra", [128, 4, D], BF16)
            wbrb = tile(es, "wbrb", [128, 4, D], BF16)
            wout = tile(es, "wout", [128, 8, D], BF16)
            wr = tile(es, "wr", [128, 8, 36], F32)
            brb = tile(es, "brb", [128, 36], F32)
            ustrict = tile(es, "ustrict", [128, 128], F32)
            onesf = tile(es, "onesf3", [128, 128], F32)
            baserow = tile(es, "baserow", [128, NE], F32)
            selsum = tile(es, "selsum", [128, NE], F32)
            rank_all = tile(es, "rank_all", [128, 32, NE], F32)
            t1_all = tile(es, "t1_all", [128, 32, NE], F32)
            t2_all = tile(es, "t2_all", [128, 32, NE], F32)
            big = tile(es, "big", [128, NBLK * NE], F32)
            big2 = tile(es, "big2", [128, NBLK * NE], F32)
            dma("gpsimd", wbra.t[:], wbra_d.rearrange("(kc p) c -> p kc c", p=128), writes=[wbra])
            dma("gpsimd", wbrb.t[:], wbrb_d.rearrange("(kc p) c -> p kc c", p=128), writes=[wbrb])
            dma("gpsimd", wout.t[:], wout_d.rearrange("(kc p) c -> p kc c", p=128), writes=[wout])
            dma("sync", wr.t[:], wr_d.rearrange("(kc p) c -> p kc c", p=128), writes=[wr])
            dma("sync", brb.t[:], br_d[0:1, :].partition_broadcast(128), writes=[brb])
            dma("sync", ustrict.t[:], ustrict_d, writes=[ustrict])
            dma("sync", onesf.t[:], onesf_d, writes=[onesf])
            op("vector", lambda: nc.vector.memset(selsum.t[:], 0.0), writes=[selsum])
            oat = [tile(es, f"oat{i}", [128, 4, 512], BF16) for i in range(2)]
            obt = [tile(es, f"obt{i}", [128, 4, 512], BF16) for i in range(2)]
            gat = [tile(es, f"gat{i}", [128, 8, 512], BF16) for i in range(2)]
            gbt = [tile(es, f"gbt{i}", [128, 8, 512], BF16) for i in range(2)]
            m1 = tile(es, "m1", [128, 512], F32)
            m2 = tile(es, "m2", [128, 512], F32)
            mg = tile(es, "mg", [128, 8, 512], BF16)
            xt_ = [tile(es, f"x3t{i}", [128, D], F32) for i in range(2)]
            x1t = [tile(es, f"x1t{i}", [128, D], F32) for i in range(2)]
            tmpx = tile(es, "tmpx", [128, D], F32)
            hf = tile(es, "h2f0", [128, D], F32)
            h2f = tile(es, "h2f", [128, D], F32)
            h2b = [tile(es, f"h2b{i}", [128, D], BF16) for i in range(2)]
            junk = tile(es, "junk3", [128, D], BF16)
            ss = tile(es, "ss3", [128, 1], F32)
            std = tile(es, "std3", [128, 1], F32)
            rstd = tile(es, "rstd3", [128, 1], F32)
            h2T = tile(es, "h2T", [128, 8, 128], F32)
            lg_ = [tile(es, f"lg{i}", [128, 36], F32) for i in range(2)]
            sm = {n: tile(es, "sm_" + n, [128, w], F32) for n, w in
                  [("gmax", 1), ("ngmax", 1), ("gexp", 4), ("gsum", 1), ("gw", 1), ("goh", 4), ("pen", 4), ("em", 32),
                   ("t8", 8), ("sel", 32), ("t1", 32), ("t2", 32), ("dd", 1), ("ex", 1), ("den", 1), ("rr", 1),
                   ("dest", 32), ("jk", 32), ("dAf", 1), ("dBf", 1)]}
            pA = ptile(es, "p3A", [128, 512], F32)
            pB = ptile(es, "p3B", [128, 512], F32)
            py = [ptile(es, f"p3y{i}", [128, 512], F32) for i in range(2)]
            ptf = [ptile(es, f"p3t{i}", [128, 512], F32) for i in range(2)]
            plg = ptile(es, "p3lg", [128, 512], F32)
            prk = ptile(es, "p3rk", [128, 512], F32)

            def load_group(g):
                i = g % 2
                sl = slice(g * 512, (g + 1) * 512)
                dma("sync", oat[i].t[:], oaT_s.rearrange("(kc p) t -> p kc t", p=128)[:, :, sl], reads=[dB["oaT"]], writes=[oat[i]])
                dma("sync", obt[i].t[:], obT_s.rearrange("(kc p) t -> p kc t", p=128)[:, :, sl], reads=[dB["obT"]], writes=[obt[i]])
                dma("sync", gat[i].t[:], gaT_s.rearrange("(kc p) t -> p kc t", p=128)[:, :, sl], reads=[dB["gaT"]], writes=[gat[i]])
                dma("sync", gbt[i].t[:], gbT_s.rearrange("(kc p) t -> p kc t", p=128)[:, :, sl], reads=[dB["gbT"]], writes=[gbt[i]])
            def tile_body(g, tt):
                k = g * 4 + tt
                tok0 = k * 128
                xt = xt_[k % 2]
                X1 = x1t[k % 2]
                H2B = h2b[k % 2]
                dma("sync", xt.t[:], x_d[tok0:tok0 + 128, :], writes=[xt])
                for half in range(2):
                    PY = py[half]

                    def mmy():
                        for kc in range(8):
                            i = nc.tensor.matmul(PY.t[:], lhsT=mg.t[:, kc, tt * 128:(tt + 1) * 128],
                                                 rhs=wout.t[:, kc, half * 512:(half + 1) * 512], start=(kc == 0), stop=(kc == 7))
                        return i
                    op("tensor", mmy, reads=[mg, wout], writes=[PY])
                    op("vector", lambda: nc.vector.tensor_tensor(out=tmpx.t[:, half * 512:(half + 1) * 512], in0=PY.t[:],
                                                                 in1=gtA.t[:, half * 512:(half + 1) * 512], op=ALU.mult),
                       reads=[PY, gtA], writes=[tmpx])
                op("gpsimd", lambda: nc.gpsimd.tensor_tensor(out=X1.t[:], in0=tmpx.t[:], in1=xt.t[:], op=ALU.add),
                   reads=[tmpx, xt], writes=[X1])
                dma("scalar", x1_s[tok0:tok0 + 128, :], X1.t[:], reads=[X1], writes=[dB["x1"]])
                norm_mod(X1, A2, B2, hf, None, junk, ss, std, rstd)
                op("vector", lambda: nc.vector.tensor_tensor(out=h2f.t[:], in0=hf.t[:], in1=B2.t[:], op=ALU.add),
                   reads=[hf, B2], writes=[h2f])
                op("gpsimd", lambda: nc.gpsimd.tensor_copy(out=H2B.t[:], in_=h2f.t[:]), reads=[h2f], writes=[H2B])
                for half in range(2):
                    PT = ptf[half]

                    def tr():
                        for c in range(4):
                            kc = half * 4 + c
                            i = nc.tensor.transpose(out=PT.t[:, c * 128:(c + 1) * 128], in_=h2f.t[:, kc * 128:(kc + 1) * 128],
                                                    identity=identf.t[:])
                        return i
                    op("tensor", tr, reads=[h2f, identf], writes=[PT])
                    op("scalar", lambda: nc.scalar.activation(out=h2T.t[:, half * 4:(half + 1) * 4, :],
                                                              in_=PT.t[:].rearrange("p (c t) -> p c t", c=4), func=AF.Copy),
                       reads=[PT], writes=[h2T])

                def mml():
                    for kc in range(8):
                        i = nc.tensor.matmul(plg.t[:, 0:36], lhsT=h2T.t[:, kc, :], rhs=wr.t[:, kc, :], start=(kc == 0),
                                             stop=(kc == 7))
                    return i
                op("tensor", mml, reads=[h2T, wr], writes=[plg])
                V = nc.vector
                s = sm
                lg = lg_[k % 2]
                op("vector", lambda: V.tensor_tensor(out=lg.t[:], in0=plg.t[:, 0:36], in1=brb.t[:], op=ALU.add),
                   reads=[plg, brb], writes=[lg])
                yield
                op("vector", lambda: V.tensor_reduce(out=s["gmax"].t[:], in_=lg.t[:, 0:4], axis=AX.X, op=ALU.max),
                   reads=[lg], writes=[s["gmax"]])
                op("vector", lambda: V.tensor_scalar(out=s["ngmax"].t[:], in0=s["gmax"].t[:], scalar1=-1.0, scalar2=None,
                                                     op0=ALU.mult), reads=[s["gmax"]], writes=[s["ngmax"]])
                op("scalar", lambda: nc.scalar.activation(out=s["gexp"].t[:], in_=lg.t[:, 0:4], func=AF.Exp,
                                                          bias=s["ngmax"].t[:, 0:1], accum_out=s["gsum"].t[:, 0:1]),
                   reads=[lg, s["ngmax"]], writes=[s["gexp"], s["gsum"]])
                op("vector", lambda: V.reciprocal(out=s["gw"].t[:], in_=s["gsum"].t[:]), reads=[s["gsum"]], writes=[s["gw"]])
                op("vector", lambda: V.tensor_scalar(out=s["goh"].t[:], in0=lg.t[:, 0:4], scalar1=s["gmax"].t[:, 0:1],
                                                     scalar2=None, op0=ALU.is_ge), reads=[lg, s["gmax"]], writes=[s["goh"]])
                op("vector", lambda: V.tensor_scalar(out=s["pen"].t[:], in0=s["goh"].t[:], scalar1=1.0, scalar2=1e30,
                                                     op0=ALU.subtract, op1=ALU.mult), reads=[s["goh"]], writes=[s["pen"]])
                op("vector", lambda: V.tensor_tensor(out=s["em"].t[:].rearrange("p (g e) -> p g e", g=4),
                                                     in0=lg.t[:, 4:36].rearrange("p (g e) -> p g e", g=4),
                                                     in1=s["pen"].t[:, :].unsqueeze(2).to_broadcast([128, 4, 8]), op=ALU.add),
                   reads=[lg, s["pen"]], writes=[s["em"]])
                op("vector", lambda: V.max(out=s["t8"].t[:], in_=s["em"].t[:]), reads=[s["em"]], writes=[s["t8"]])
                op("vector", lambda: V.tensor_scalar(out=s["sel"].t[:], in0=s["em"].t[:], scalar1=s["t8"].t[:, 1:2],
                                                     scalar2=None, op0=ALU.is_ge), reads=[s["em"], s["t8"]], writes=[s["sel"]])
                op("vector", lambda: V.tensor_scalar(out=s["t1"].t[:], in0=s["em"].t[:], scalar1=s["t8"].t[:, 0:1],
                                                     scalar2=None, op0=ALU.is_ge), reads=[s["em"], s["t8"]], writes=[s["t1"]])
                op("vector", lambda: V.tensor_tensor(out=s["t2"].t[:], in0=s["sel"].t[:], in1=s["t1"].t[:], op=ALU.subtract),
                   reads=[s["sel"], s["t1"]], writes=[s["t2"]])
                op("vector", lambda: V.tensor_tensor(out=s["dd"].t[:], in0=s["t8"].t[:, 1:2], in1=s["t8"].t[:, 0:1],
                                                     op=ALU.subtract), reads=[s["t8"]], writes=[s["dd"]])
                op("scalar", lambda: nc.scalar.activation(out=s["ex"].t[:], in_=s["dd"].t[:], func=AF.Exp), reads=[s["dd"]],
                   writes=[s["ex"]])
                op("vector", lambda: V.tensor_scalar(out=s["den"].t[:], in0=s["ex"].t[:], scalar1=1.0, scalar2=None,
                                                     op0=ALU.add), reads=[s["ex"]], writes=[s["den"]])
                op("vector", lambda: V.reciprocal(out=s["rr"].t[:], in_=s["den"].t[:]), reads=[s["den"]], writes=[s["rr"]])
                op("vector", lambda: V.tensor_tensor(out=wA_all.t[:, k:k + 1], in0=s["rr"].t[:], in1=s["gw"].t[:], op=ALU.mult),
                   reads=[s["rr"], s["gw"]], writes=[wA_all])
                op("vector", lambda: V.tensor_tensor(out=wB_all.t[:, k:k + 1], in0=s["gw"].t[:], in1=wA_all.t[:, k:k + 1],
                                                     op=ALU.subtract), reads=[s["gw"], wA_all], writes=[wB_all])

                def mmr():
                    nc.tensor.matmul(prk.t[:, 0:32], lhsT=ustrict.t[:], rhs=s["sel"].t[:], start=True, stop=False)
                    return nc.tensor.matmul(prk.t[:, 0:32], lhsT=onesf.t[:], rhs=selsum.t[:], start=False, stop=True)
                op("tensor", mmr, reads=[ustrict, onesf, s["sel"], selsum], writes=[prk])
                op("scalar", lambda: nc.scalar.activation(out=rank_all.t[:, k, :], in_=prk.t[:, 0:32], func=AF.Copy),
                   reads=[prk], writes=[rank_all])
                op("vector", lambda: V.tensor_tensor(out=selsum.t[:], in0=selsum.t[:], in1=s["sel"].t[:], op=ALU.add),
                   reads=[selsum, s["sel"]], writes=[selsum])
                op("vector", lambda: V.tensor_copy(out=t1_all.t[:, k, :], in_=s["t1"].t[:]), reads=[s["t1"]], writes=[t1_all])
                op("vector", lambda: V.tensor_copy(out=t2_all.t[:, k, :], in_=s["t2"].t[:]), reads=[s["t2"]], writes=[t2_all])
                dma("scalar", h2_s[tok0:tok0 + 128, :], H2B.t[:], reads=[H2B], writes=[dB["h2"]])

            pend_gen = []
            load_group(0)
            for g in range(NG):
                if g + 1 < NG:
                    load_group(g + 1)
                OA, OB, GA, GB = oat[g % 2], obt[g % 2], gat[g % 2], gbt[g % 2]
                for dc in range(8):
                    def mmA():
                        for kc in range(4):
                            i = nc.tensor.matmul(pA.t[:], lhsT=wbra.t[:, kc, dc * 128:(dc + 1) * 128], rhs=OA.t[:, kc, :],
                                                 start=(kc == 0), stop=(kc == 3))
                        return i

                    def mmB():
                        for kc in range(4):
                            i = nc.tensor.matmul(pB.t[:], lhsT=wbrb.t[:, kc, dc * 128:(dc + 1) * 128], rhs=OB.t[:, kc, :],
                                                 start=(kc == 0), stop=(kc == 3))
                        return i
                    op("tensor", mmA, reads=[wbra, OA], writes=[pA])
                    op("tensor", mmB, reads=[wbrb, OB], writes=[pB])
                    op("vector", lambda: nc.vector.tensor_tensor(out=m1.t[:], in0=pA.t[:], in1=GA.t[:, dc, :], op=ALU.mult),
                       reads=[pA, GA], writes=[m1])
                    op("vector", lambda: nc.vector.tensor_tensor(out=m2.t[:], in0=pB.t[:], in1=GB.t[:, dc, :], op=ALU.mult),
                       reads=[pB, GB], writes=[m2])
                    op("gpsimd", lambda: nc.gpsimd.tensor_tensor(out=mg.t[:, dc, :], in0=m1.t[:], in1=m2.t[:], op=ALU.add),
                       reads=[m1, m2], writes=[mg])
                for tt in range(4):
                    gen = tile_body(g, tt)
                    next(gen)
                    for pg_ in pend_gen:
                        for _ in pg_:
                            pass
                    pend_gen = [gen]
            for pg_ in pend_gen:
                for _ in pg_:
                    pass
            V = nc.vector
            op("tensor", lambda: nc.tensor.matmul(prk.t[:, 0:32], lhsT=onesf.t[:], rhs=selsum.t[:], start=True, stop=True),
               reads=[onesf, selsum], writes=[prk])
            cnt = tile(es, "cnt", [128, NE], F32)
            nblk = tile(es, "nblk", [128, NE], F32)
            pend = tile(es, "pend", [128, NE], F32)
            onesr = tile(es, "onesr", [128, NE], F32)
            blke = tile(es, "blke", [128, NBLK], F32)
            pcol = tile(es, "pcol", [128, 1], F32)
            dma("sync", pcol.t[:], pcol_d, writes=[pcol])
            op("vector", lambda: V.tensor_copy(out=cnt.t[:], in_=prk.t[:, 0:32]), reads=[prk], writes=[cnt])
            op("vector", lambda: V.memset(onesr.t[:], 1.0), writes=[onesr])
            dma("sync", big.t[:, 0:NE * 32], thr_d, writes=[big])
            op("vector", lambda: V.tensor_tensor(out=big2.t[:, 0:NE * 32].rearrange("p (e m) -> p e m", e=NE),
                                                 in0=cnt.t[:, :].unsqueeze(2).to_broadcast([128, NE, 32]),
                                                 in1=big.t[:, 0:NE * 32].rearrange("p (e m) -> p e m", e=NE), op=ALU.is_gt),
               reads=[cnt, big], writes=[big2])
            op("vector", lambda: V.tensor_reduce(out=nblk.t[:], in_=big2.t[:, 0:NE * 32].rearrange("p (e m) -> p e m", e=NE),
                                                 axis=AX.X, op=ALU.add), reads=[big2], writes=[nblk])
            op("vector", lambda: V.tensor_tensor_scan(out=pend.t[:], data0=onesr.t[:], data1=nblk.t[:], initial=0.0,
                                                      op0=ALU.mult, op1=ALU.add), reads=[onesr, nblk], writes=[pend])
            op("vector", lambda: V.tensor_tensor(out=baserow.t[:], in0=pend.t[:], in1=nblk.t[:], op=ALU.subtract),
               reads=[pend, nblk], writes=[baserow])
            op("vector", lambda: V.tensor_scalar(out=baserow.t[:], in0=baserow.t[:], scalar1=float(RB), scalar2=None,
                                                 op0=ALU.mult), reads=[baserow], writes=[baserow])
            dma("sync", big.t[:], jv_d, writes=[big])
            op("vector", lambda: V.tensor_tensor(out=big2.t[:].rearrange("p (j e) -> p j e", j=NBLK),
                                                 in0=pend.t[:, :].unsqueeze(1).to_broadcast([128, NBLK, NE]),
                                                 in1=big.t[:].rearrange("p (j e) -> p j e", j=NBLK), op=ALU.is_le),
               reads=[pend, big], writes=[big2])
            op("vector", lambda: V.tensor_reduce(out=blke.t[:], in_=big2.t[:].rearrange("p (j e) -> p j e", j=NBLK),
                                                 axis=AX.X, op=ALU.add), reads=[big2], writes=[blke])
            op("vector", lambda: V.tensor_scalar(out=blke.t[:], in0=blke.t[:], scalar1=float(NE), scalar2=128.0,
                                                 op0=ALU.min, op1=ALU.mult), reads=[blke], writes=[blke])
            op("vector", lambda: V.tensor_scalar(out=blke.t[:], in0=blke.t[:], scalar1=pcol.t[:, 0:1], scalar2=None,
                                                 op0=ALU.add), reads=[blke, pcol], writes=[blke])
            op("vector", lambda: V.tensor_copy(out=widx.t[:], in_=blke.t[:]), reads=[blke], writes=[widx])
            r3 = lambda tl: tl.t[:].rearrange("p k e -> p (k e)")
            op("vector", lambda: V.tensor_tensor(out=rank_all.t[:], in0=rank_all.t[:],
                                                 in1=baserow.t[:, :].unsqueeze(1).to_broadcast([128, 32, NE]), op=ALU.add),
               reads=[rank_all, baserow], writes=[rank_all])
            op("vector", lambda: V.tensor_tensor(out=t1_all.t[:], in0=t1_all.t[:], in1=rank_all.t[:], op=ALU.mult),
               reads=[t1_all, rank_all], writes=[t1_all])
            op("vector", lambda: V.tensor_tensor(out=t2_all.t[:], in0=t2_all.t[:], in1=rank_all.t[:], op=ALU.mult),
               reads=[t2_all, rank_all], writes=[t2_all])
            dAf = tile(es, "dAf_all", [128, 32], F32)
            dBf = tile(es, "dBf_all", [128, 32], F32)
            op("vector", lambda: V.tensor_reduce(out=dAf.t[:], in_=t1_all.t[:], axis=AX.X, op=ALU.add), reads=[t1_all], writes=[dAf])
            op("vector", lambda: V.tensor_reduce(out=dBf.t[:], in_=t2_all.t[:], axis=AX.X, op=ALU.add), reads=[t2_all], writes=[dBf])
            op("vector", lambda: V.tensor_copy(out=dA_all.t[:], in_=dAf.t[:]), reads=[dAf], writes=[dA_all])
            op("vector", lambda: V.tensor_copy(out=dB_all.t[:], in_=dBf.t[:]), reads=[dBf], writes=[dB_all])
            for k in range(32):
                H2B = h2b[k % 2]
                dma("sync", H2B.t[:], h2_s[k * 128:(k + 1) * 128, :], reads=[dB["h2"]], writes=[H2B])
                dma("gpsimd", xg_s[:, :], H2B.t[:], reads=[H2B, dA_all], writes=[dB["xg"]],
                    out_off=bass.IndirectOffsetOnAxis(ap=dA_all.t[:, k:k + 1], axis=0))
                dma("gpsimd", xg_s[:, :], H2B.t[:], reads=[H2B, dB_all], writes=[dB["xg"]],
                    out_off=bass.IndirectOffsetOnAxis(ap=dB_all.t[:, k:k + 1], axis=0))
            T.barrier()
        if STOP_AFTER <= 4:
            T.barrier()
            return nc

        with ExitStack() as es:
            NW = 3
            wg = [tile(es, f"wg{i}", [128, 8 * 512], BF16) for i in range(NW)]
            wu = [tile(es, f"wu{i}", [128, 8 * 512], BF16) for i in range(NW)]
            wd = [tile(es, f"wd{i}", [128, 4 * D], BF16) for i in range(NW)]
            xr = [tile(es, f"xr{i}", [128, D], BF16) for i in range(3)]
            xT = [tile(es, f"xT{i}", [128, 8, RB], BF16) for i in range(2)]
            sg = [tile(es, f"sg{i}", [128, RB], F32) for i in range(2)]
            hid = [tile(es, f"hid{i}", [128, 4, RB], BF16) for i in range(2)]
            yt = [tile(es, f"yt{i}", [128, D], F32) for i in range(3)]
            ptr = [ptile(es, f"p4t{i}", [128, 1024], BF16) for i in range(2)]
            pG = [ptile(es, f"p4g{i}", [128, 512], F32) for i in range(2)]
            pU = [ptile(es, f"p4u{i}", [128, 512], F32) for i in range(2)]
            pY = [ptile(es, f"p4y{i}", [128, 512], F32) for i in range(2)]
            NRT = RB // 128
            bc_reg = nc.gpsimd.to_reg(NE * 128 - 1)
            for wt in wg + wu + wd:
                op("gpsimd", lambda: nc.gpsimd.memset(wt.t[:], 0.0), writes=[wt])

            def load_w(j):
                i = j % NW
                off = bass.IndirectOffsetOnAxis(ap=widx.t[:, j:j + 1], axis=0)
                kw = dict(bounds_check=bc_reg, oob_is_err=False)
                dma("gpsimd", wg[i].t[:], wg_d[:, :], reads=[widx], writes=[wg[i]], in_off=off, **kw)
                dma("gpsimd", wu[i].t[:], wu_d[:, :], reads=[widx], writes=[wu[i]], in_off=off, **kw)
                dma("gpsimd", wd[i].t[:], wd_d[:, :], reads=[widx], writes=[wd[i]], in_off=off, **kw)

            def stT(j):
                XT = xT[j % 2]
                for r in range(NRT):
                    c_ = j * NRT + r
                    XR, PT = xr[c_ % 3], ptr[c_ % 2]
                    row0 = j * RB + r * 128
                    dma("sync", XR.t[:], xg_s[row0:row0 + 128, :], reads=[dB["xg"]], writes=[XR])

                    def tr():
                        for kc in range(8):
                            i = nc.tensor.transpose(out=PT.t[:, kc * 128:(kc + 1) * 128], in_=XR.t[:, kc * 128:(kc + 1) * 128],
                                                    identity=identb.t[:])
                        return i
                    op("tensor", tr, reads=[XR, identb], writes=[PT])
                    op("scalar", lambda: nc.scalar.activation(out=XT.t[:, :, r * 128:(r + 1) * 128],
                                                              in_=PT.t[:].rearrange("p (kc t) -> p kc t", kc=8), func=AF.Copy),
                       reads=[PT], writes=[XT])

            def stH(j):
                XT, HID = xT[j % 2], hid[j % 2]
                WG = wg[j % NW].t[:].rearrange("p (kc f) -> p kc f", kc=8)
                WU = wu[j % NW].t[:].rearrange("p (kc f) -> p kc f", kc=8)
                for fc in range(4):
                    G, U, SG = pG[fc % 2], pU[fc % 2], sg[fc % 2]

                    def mmg():
                        for kc in range(8):
                            i = nc.tensor.matmul(G.t[:, 0:RB], lhsT=WG[:, kc, fc * 128:(fc + 1) * 128], rhs=XT.t[:, kc, :],
                                                 start=(kc == 0), stop=(kc == 7))
                        return i

                    def mmu():
                        for kc in range(8):
                            i = nc.tensor.matmul(U.t[:, 0:RB], lhsT=WU[:, kc, fc * 128:(fc + 1) * 128], rhs=XT.t[:, kc, :],
                                                 start=(kc == 0), stop=(kc == 7))
                        return i
                    op("tensor", mmg, reads=[wg[j % NW], XT], writes=[G])
                    op("tensor", mmu, reads=[wu[j % NW], XT], writes=[U])
                    op("scalar", lambda: nc.scalar.activation(out=SG.t[:], in_=G.t[:, 0:RB], func=AF.Silu), reads=[G], writes=[SG])
                    op("vector", lambda: nc.vector.tensor_tensor(out=HID.t[:, fc, :], in0=SG.t[:], in1=U.t[:, 0:RB], op=ALU.mult),
                       reads=[SG, U], writes=[HID])

            def stD(j):
                HID = hid[j % 2]
                WD = wd[j % NW].t[:].rearrange("p (fc d) -> p fc d", fc=4)
                for r in range(NRT):
                    YT = yt[(j * NRT + r) % 3]
                    for half in range(2):
                        PY = pY[half]

                        def mmy():
                            for fc in range(4):
                                i = nc.tensor.matmul(PY.t[:], lhsT=HID.t[:, fc, r * 128:(r + 1) * 128],
                                                     rhs=WD[:, fc, half * 512:(half + 1) * 512], start=(fc == 0), stop=(fc == 3))
                            return i
                        op("tensor", mmy, reads=[HID, wd[j % NW]], writes=[PY])
                        if half == 0:
                            op("scalar", lambda: nc.scalar.activation(out=YT.t[:, 0:512], in_=PY.t[:], func=AF.Copy), reads=[PY],
                               writes=[YT])
                        else:
                            op("vector", lambda: nc.vector.tensor_copy(out=YT.t[:, 512:1024], in_=PY.t[:]), reads=[PY], writes=[YT])
                    row0 = j * RB + r * 128
                    dma("scalar", y_s[row0:row0 + 128, :], YT.t[:], reads=[YT], writes=[dB["y"]])

            load_w(0)
            for step in range(NBLK + 2):
                if step < NBLK:
                    stT(step)
                if 0 <= step - 1 < NBLK:
                    stH(step - 1)
                if 0 <= step - 2 < NBLK:
                    stD(step - 2)
                if step + 1 < NBLK:
                    load_w(step + 1)
            T.barrier()
        if STOP_AFTER <= 5:
            T.barrier()
            return nc

        with ExitStack() as es:
            ya = [tile(es, f"ya{i}", [128, D], F32) for i in range(3)]
            yb = [tile(es, f"yb{i}", [128, D], F32) for i in range(3)]
            x1t = [tile(es, f"x5t{i}", [128, D], F32) for i in range(3)]
            mo_ = [tile(es, f"mo{i}", [128, D], F32) for i in range(2)]
            mo2_ = [tile(es, f"mo2{i}", [128, D], F32) for i in range(2)]
            ot = [tile(es, f"ot{i}", [128, D], F32) for i in range(2)]
            NB5 = 3

            def fetch(k):
                tok0 = k * 128
                dma("gpsimd", ya[k % NB5].t[:], y_s[:, :], reads=[dB["y"], dA_all], writes=[ya[k % NB5]],
                    in_off=bass.IndirectOffsetOnAxis(ap=dA_all.t[:, k:k + 1], axis=0))
                dma("gpsimd", yb[k % NB5].t[:], y_s[:, :], reads=[dB["y"], dB_all], writes=[yb[k % NB5]],
                    in_off=bass.IndirectOffsetOnAxis(ap=dB_all.t[:, k:k + 1], axis=0))
                dma("sync", x1t[k % NB5].t[:], x1_s[tok0:tok0 + 128, :], reads=[dB["x1"]], writes=[x1t[k % NB5]])
            fetch(0)
            fetch(1)
            for k in range(32):
                if k + 2 < 32:
                    fetch(k + 2)
                YA, YB, X1, OT = ya[k % NB5], yb[k % NB5], x1t[k % NB5], ot[k % 2]
                mo, mo2 = mo_[k % 2], mo2_[k % 2]
                tok0 = k * 128
                op("scalar", lambda: nc.scalar.activation(out=mo.t[:], in_=YA.t[:], func=AF.Copy, scale=wA_all.t[:, k:k + 1]),
                   reads=[YA, wA_all], writes=[mo])
                op("vector", lambda: nc.vector.scalar_tensor_tensor(out=mo2.t[:], in0=YB.t[:], scalar=wB_all.t[:, k:k + 1],
                                                                    in1=mo.t[:], op0=ALU.mult, op1=ALU.add),
                   reads=[YB, wB_all, mo], writes=[mo2])
                op("vector", lambda: nc.vector.tensor_tensor(out=mo.t[:], in0=mo2.t[:], in1=gtF.t[:], op=ALU.mult),
                   reads=[mo2, gtF], writes=[mo])
                op("gpsimd", lambda: nc.gpsimd.tensor_tensor(out=OT.t[:], in0=mo.t[:], in1=X1.t[:], op=ALU.add),
                   reads=[mo, X1], writes=[OT])
                dma("scalar", out_d[tok0:tok0 + 128, :], OT.t[:], reads=[OT], writes=[dB["out"]])
            T.barrier()
    return nc


_CONSTS = None


def host_consts(rel_bias):
    bf = ml_dtypes.bfloat16
    c = {}
    c["identb"] = np.eye(128, dtype=np.float32).astype(bf)
    c["identf"] = np.eye(128, dtype=np.float32)
    bd = np.zeros((128, 128), np.float32)
    bd[:64, :64] = 1.0 / 64
    bd[64:, 64:] = 1.0 / 64
    c["blkdiag"] = bd
    em = np.zeros((16, 16, 128), np.float32)
    for n in range(16):
        em[n, n, :] = 1.0
    c["emat"] = em.reshape(16, 16 * 128).astype(bf)
    j = np.arange(128)
    c["negtri"] = ((j[:, None] >= j[None, :]).astype(np.float32)).astype(bf)
    c["negones"] = (np.ones((128, 128), np.float32)).astype(bf)
    qi = np.arange(512)
    ms = np.zeros((128, 4, 512), np.float32)
    for jj in range(4):
        ms[:, jj, :] = ((jj * 128 + j[:, None]) < qi[None, :]).astype(np.float32)
    c["masks"] = ms.reshape(128, 4 * 512).astype(bf)
    c["ustrict"] = (j[:, None] < j[None, :]).astype(np.float32)
    c["onesf"] = np.ones((128, 128), np.float32)
    c["baserow"] = np.zeros((128, NE), np.float32)
    c["thr"] = np.broadcast_to((np.arange(32) * RB).astype(np.float32)[None, None, :], (128, NE, 32)).reshape(128, NE * 32).copy()
    c["jv"] = np.broadcast_to(np.arange(NBLK).astype(np.float32)[None, :, None], (128, NBLK, NE)).reshape(128, NBLK * NE).copy()
    c["pcol"] = np.arange(128, dtype=np.float32).reshape(128, 1)
    bt = np.zeros((H, 128, NOFF, 512), np.float32)
    for oi in range(NOFF):
        delta = -384 + 128 * oi
        d = delta + qi[None, :] - j[:, None]
        bk = rel_bucket_np(d)
        g = rel_bias[:, bk]
        g = np.where((d >= 0)[None], g, np.float32(-BIG))
        bt[:, :, oi, :] = g
    c["biast"] = bt.reshape(H, 128, NOFF * 512)
    c["blkind"] = (np.arange(S)[None, :] // 256 == np.arange(16)[:, None]).astype(np.float32).astype(bf)
    c["diagm"] = ((j[None, :] + j[:, None] - 127) <= 0).astype(np.float32)
    c["antii"] = np.eye(128, dtype=np.float32)[::-1].copy().astype(bf)
    c["bias31"] = np.broadcast_to(rel_bias[:, 31][None, :], (128, H)).astype(np.float32).copy()
    return c


def make_in_maps(x, c, w_ada, b_ada, g_mix, w_in, g_q, g_k, rel_bias, w_br_moba, w_br_sb, w_out,
                 g_ffn, w_rg, b_rg, w_re, b_re, w_gate, w_up, w_down, cores=range(8)):
    f = lambda a: np.ascontiguousarray(np.asarray(a, dtype=np.float32))
    x = f(x); c = f(c)
    consts = host_consts(f(rel_bias))
    gqk = np.stack([np.tile(f(g_q)[0], 2), np.tile(f(g_k)[0], 2)], axis=1)
    shared = {
        "w_ada": f(w_ada)[0], "b_ada": f(b_ada), "g_mix": f(g_mix), "w_in": f(w_in)[0], "gqk": np.ascontiguousarray(gqk),
        "w_br_moba": f(w_br_moba)[0], "w_br_sb": f(w_br_sb)[0], "w_out": f(w_out)[0], "g_ffn": f(g_ffn),
        "w_r": np.ascontiguousarray(np.concatenate([f(w_rg)[0], f(w_re)[0]], axis=1)),
        "b_r": np.ascontiguousarray(np.concatenate([f(b_rg), f(b_re)], axis=1)),
        "w_gate_r": np.ascontiguousarray(f(w_gate)[0].reshape(NE, 8, 128, 512).transpose(0, 2, 1, 3)).reshape(NE * 128, 8 * 512),
        "w_up_r": np.ascontiguousarray(f(w_up)[0].reshape(NE, 8, 128, 512).transpose(0, 2, 1, 3)).reshape(NE * 128, 8 * 512),
        "w_down_r": np.ascontiguousarray(f(w_down)[0].reshape(NE, 4, 128, D).transpose(0, 2, 1, 3)).reshape(NE * 128, 4 * D),
    }
    shared.update(consts)
    in_maps = []
    for b in cores:
        m = dict(shared)
        m["x"] = x[b]
        m["ccol"] = np.ascontiguousarray(c[b].reshape(8, 128).T)
        in_maps.append(m)
    return in_maps


def kernel(**inputs):
    in_maps = make_in_maps(**inputs)
    nc = build_program()
    res = run_bass_kernel_spmd(nc, in_maps, core_ids=list(range(8)))
    return np.stack([res.results[b]["out"] for b in range(8)], axis=0).astype(np.float32)
```

```python
import math
from contextlib import ExitStack
import numpy as np
import ml_dtypes
import concourse.bass as bass
import concourse.mybir as mybir
from concourse.bass_utils import run_bass_kernel_spmd

F32 = mybir.dt.float32
BF16 = mybir.dt.bfloat16
I32 = mybir.dt.int32
AF = mybir.ActivationFunctionType
ALU = mybir.AluOpType
AX = mybir.AxisListType

S = 4096
D = 1024
H = 8
DH = 64
NG = 8
NE = 32
RB = 256
NBLK = (2 * S) // RB + NE
NROWS = NBLK * RB
NOFF = 11
BIG = 30000.0
SB_WINDOW = None
DEBUG = False
STOP_AFTER = 99


class Buf:
    def __init__(self, multi=False):
        self.ws = {}
        self.r = {}
        self.multi = multi


class Tl(Buf):
    def __init__(self, t):
        super().__init__()
        self.t = t


class Tracker:
    K = 12

    def __init__(self, nc, es):
        self.nc = nc
        self.eng = {}
        for name in ["tensor", "vector", "scalar", "gpsimd", "sync"]:
            sem = es.enter_context(nc.semaphore("c_" + name))
            self.eng[name] = dict(e=getattr(nc, name), sem=sem, n=0, seen={})
        self.dq = {}
        for q in ["sync", "gpsimd", "scalar"]:
            sems = [es.enter_context(nc.semaphore(f"d_{q}_{i}")) for i in range(self.K)]
            self.dq[q] = dict(sems=sems, cnt=[0] * self.K, toks=[None] * self.K, i=0)

    def wait(self, en, tok):
        if tok is None:
            return
        key, sem, val = tok
        E = self.eng[en]
        if E["seen"].get(key, 0) >= val:
            return
        if key == en and en in ("tensor", "sync"):
            return
        E["e"].wait_ge(sem, val)
        E["seen"][key] = val

    def _deps(self, reads, writes):
        deps = []
        for b in reads:
            deps.extend(b.ws.values())
        for b in writes:
            if not b.multi:
                deps.extend(b.ws.values())
            deps.extend(b.r.values())
        return deps

    def _mark(self, tok, reads, writes):
        for b in reads:
            b.r[tok[0]] = tok
        for b in writes:
            if b.multi:
                b.ws[tok[0]] = tok
            else:
                b.ws = {tok[0]: tok}
            b.r = {}

    def op(self, en, fn, reads=(), writes=()):
        for d in self._deps(reads, writes):
            self.wait(en, d)
        inst = fn()
        E = self.eng[en]
        E["n"] += 1
        inst.then_inc(E["sem"], 1)
        tok = (en, E["sem"], E["n"])
        E["seen"][en] = max(E["seen"].get(en, 0), 0)
        self._mark(tok, reads, writes)
        return tok

    def dma(self, q, out, in_, reads=(), writes=(), out_off=None, in_off=None, **ikw):
        Q = self.dq[q]
        i = Q["i"] % self.K
        Q["i"] += 1
        self.wait(q, Q["toks"][i])
        for d in self._deps(reads, writes):
            self.wait(q, d)
        e = self.eng[q]["e"]
        if out_off is None and in_off is None:
            inst = e.dma_start(out=out, in_=in_)
        else:
            inst = e.indirect_dma_start(out=out, out_offset=out_off, in_=in_, in_offset=in_off, **ikw)
        Q["cnt"][i] += 16
        inst.then_inc(Q["sems"][i], 16)
        tok = ((q, "dma", i), Q["sems"][i], Q["cnt"][i])
        Q["toks"][i] = tok
        self._mark(tok, reads, writes)
        return tok

    def barrier(self):
        toks = [(en, E["sem"], E["n"]) for en, E in self.eng.items() if E["n"] > 0]
        for Q in self.dq.values():
            toks += [t for t in Q["toks"] if t is not None]
        for en in self.eng:
            for t in toks:
                if t[0] == en and en in ("tensor", "sync"):
                    continue
                self.wait(en, t)


def rel_bucket_np(d):
    n = np.maximum(d, 0)
    me = 16
    nf = np.maximum(n, 1).astype(np.float32)
    large = me + (np.log(nf / np.float32(me)) / np.float32(math.log(1024 / me)) * np.float32(32 - me)).astype(np.int32)
    large = np.minimum(large, 31)
    return np.where(n < me, n, large)


def build_program():
    nc = bass.Bass("TRN2", target_bir_lowering=False)
    dbgk = "ExternalOutput" if DEBUG else "Internal"

    def din(name, shape, dt=F32):
        return nc.dram_tensor(name, list(shape), dt, kind="ExternalInput").ap()

    def dscr(name, shape, dt):
        return nc.dram_tensor(name, list(shape), dt, kind=dbgk).ap()

    x_d = din("x", [S, D])
    ccol_d = din("ccol", [128, 8])
    wada_d = din("w_ada", [D, 6 * D])
    bada_d = din("b_ada", [1, 6 * D])
    gmix_d = din("g_mix", [1, D])
    win_d = din("w_in", [D, 5120])
    gqk_d = din("gqk", [128, 2])
    wbra_d = din("w_br_moba", [512, D])
    wbrb_d = din("w_br_sb", [512, D])
    wout_d = din("w_out", [D, D])
    gffn_d = din("g_ffn", [1, D])
    wr_d = din("w_r", [D, 36])
    br_d = din("b_r", [1, 36])
    wg_d = din("w_gate_r", [NE * 128, 8 * 512])
    wu_d = din("w_up_r", [NE * 128, 8 * 512])
    wd_d = din("w_down_r", [NE * 128, 4 * D])
    thr_d = din("thr", [128, NE * 32])
    jv_d = din("jv", [128, NBLK * NE])
    pcol_d = din("pcol", [128, 1])
    identb_d = din("identb", [128, 128], BF16)
    identf_d = din("identf", [128, 128])
    blkdiag_d = din("blkdiag", [128, 128])
    emat_d = din("emat", [16, 16 * 128], BF16)
    negtri_d = din("negtri", [128, 128], BF16)
    negones_d = din("negones", [128, 128], BF16)
    masks_d = din("masks", [128, 4 * 512], BF16)
    ustrict_d = din("ustrict", [128, 128])
    onesf_d = din("onesf", [128, 128])
    baserow_d = din("baserow", [128, NE])
    biast_d = din("biast", [H, 128, NOFF * 512])
    bias31_d = din("bias31", [128, H])
    blkind_d = din("blkind", [16, S], BF16)
    diagm_d = din("diagm", [128, 128])
    antii_d = din("antii", [128, 128], BF16)

    out_d = nc.dram_tensor("out", [S, D], F32, kind="ExternalOutput").ap()

    mod_s = dscr("mod_s", [1, 6 * D], F32)
    qaT_s = dscr("qaT_s", [512, S], BF16)
    kaT_s = dscr("kaT_s", [512, S], BF16)
    qbT_s = dscr("qbT_s", [512, S], BF16)
    kbT_s = dscr("kbT_s", [512, S], BF16)
    va_s = dscr("va_s", [S, H * 65], BF16)
    vb_s = dscr("vb_s", [S, 512], BF16)
    gaT_s = dscr("gaT_s", [D, S], BF16)
    gbT_s = dscr("gbT_s", [D, S], BF16)
    mT_s = dscr("mT_s", [128, S], BF16)
    oaT_s = dscr("oaT_s", [512, S], BF16)
    obT_s = dscr("obT_s", [512, S], BF16)
    x1_s = dscr("x1_s", [S, D], F32)
    xg_s = dscr("xg_s", [NROWS, D], BF16)
    h2_s = dscr("h2_s", [S, D], BF16)
    dbgf_s = dscr("dbgf_s", [128, 1032], F32)
    dbgb_s = dscr("dbgb_s", [128, 256], BF16)
    y_s = dscr("y_s", [NROWS, D], F32)

    top = ExitStack()
    with top:
        T = Tracker(nc, top)
        build_program.T = T
        op, dma = T.op, T.dma

        def tile(es, name, shape, dt):
            return Tl(es.enter_context(nc.sbuf_tensor("sb_" + name, list(shape), dt)))

        def ptile(es, name, shape, dt):
            return Tl(es.enter_context(nc.psum_tensor("ps_" + name, list(shape), dt)))

        dB = {n: Buf(multi=True) for n in ["mod", "qaT", "kaT", "qbT", "kbT", "va", "vb", "gaT", "gbT", "mT", "oaT", "obT",
                                 "x1", "xg", "y", "out", "h2"]}

        A1 = tile(top, "A1", [128, D], F32)
        B1 = tile(top, "B1", [128, D], F32)
        gtA = tile(top, "gtA", [128, D], F32)
        A2 = tile(top, "A2", [128, D], F32)
        B2 = tile(top, "B2", [128, D], F32)
        gtF = tile(top, "gtF", [128, D], F32)
        identb = tile(top, "identb", [128, 128], BF16)
        identf = tile(top, "identf", [128, 128], F32)
        epsc = tile(top, "epsc", [128, 1], F32)
        onec = tile(top, "onec", [128, 1], F32)
        dA_all = tile(top, "dA_all", [128, 32], I32)
        dB_all = tile(top, "dB_all", [128, 32], I32)
        wA_all = tile(top, "wA_all", [128, 32], F32)
        wB_all = tile(top, "wB_all", [128, 32], F32)
        widx = tile(top, "widx", [128, NBLK], I32)
        dma("sync", identb.t[:], identb_d, writes=[identb])
        dma("sync", identf.t[:], identf_d, writes=[identf])
        op("vector", lambda: nc.vector.memset(epsc.t[:], 1e-6), writes=[epsc])
        op("vector", lambda: nc.vector.memset(onec.t[:], 1.0), writes=[onec])

        es_win = ExitStack()
        win = tile(es_win, "win", [128, 8, 5120], BF16)
        winv = win_d.rearrange("(kc p) c -> p kc c", p=128)
        for kc in range(8):
            dma("gpsimd", win.t[:, kc, :], winv[:, kc, :], writes=[win])

        with ExitStack() as es:
            ccol = tile(es, "ccol", [128, 8], F32)
            silc = tile(es, "silc", [128, 8], F32)
            bada = tile(es, "bada", [1, 6 * D], F32)
            modrow = tile(es, "modrow", [1, 6 * D], F32)
            wa = [tile(es, f"wa{i}", [128, 8, 512], F32) for i in range(2)]
            pm = [ptile(es, f"pm{i}", [128, 512], F32) for i in range(2)]
            gm = tile(es, "gm", [128, D], F32)
            sc = tile(es, "sc", [128, D], F32)
            dma("sync", ccol.t[:], ccol_d, writes=[ccol])
            dma("sync", bada.t[:], bada_d, writes=[bada])
            op("scalar", lambda: nc.scalar.activation(out=silc.t[:], in_=ccol.t[:], func=AF.Silu),
               reads=[ccol], writes=[silc])
            wv = wada_d.rearrange("(kc p) c -> p kc c", p=128)
            for j in range(12):
                W = wa[j % 2]
                P = pm[j % 2]
                dma("sync", W.t[:], wv[:, :, j * 512:(j + 1) * 512], writes=[W])

                def mm():
                    for kc in range(8):
                        i = nc.tensor.matmul(P.t[0:1, :], lhsT=silc.t[:, kc:kc + 1], rhs=W.t[:, kc, :],
                                             start=(kc == 0), stop=(kc == 7))
                    return i
                op("tensor", mm, reads=[W, silc], writes=[P])
                op("vector", lambda: nc.vector.tensor_tensor(out=modrow.t[0:1, j * 512:(j + 1) * 512], in0=P.t[0:1, :],
                                                             in1=bada.t[0:1, j * 512:(j + 1) * 512], op=ALU.add),
                   reads=[P, bada], writes=[modrow])
            dma("sync", mod_s, modrow.t[:], reads=[modrow], writes=[dB["mod"]])

            def bc(dst, i):
                dma("sync", dst.t[:], mod_s[0:1, i * D:(i + 1) * D].partition_broadcast(128), reads=[dB["mod"]],
                    writes=[dst])
            bc(B1, 0)
            bc(gtA, 2)
            bc(B2, 3)
            bc(gtF, 5)
            dma("sync", gm.t[:], gmix_d[0:1, :].partition_broadcast(128), writes=[gm])
            bc(sc, 1)
            op("vector", lambda: nc.vector.scalar_tensor_tensor(out=A1.t[:], in0=sc.t[:], scalar=1.0, in1=gm.t[:],
                                                                op0=ALU.add, op1=ALU.mult),
               reads=[sc, gm], writes=[A1])
            dma("sync", gm.t[:], gffn_d[0:1, :].partition_broadcast(128), writes=[gm])
            bc(sc, 4)
            op("vector", lambda: nc.vector.scalar_tensor_tensor(out=A2.t[:], in0=sc.t[:], scalar=1.0, in1=gm.t[:],
                                                                op0=ALU.add, op1=ALU.mult),
               reads=[sc, gm], writes=[A2])
            T.barrier()
        if STOP_AFTER <= 0:
            T.barrier()
            return nc

        def norm_mod(xt, A, B, hf, hb, junk, ss, std, rstd):
            op("scalar", lambda: nc.scalar.activation(out=junk.t[:], in_=xt.t[:], func=AF.Square,
                                                      accum_out=ss.t[:, 0:1]), reads=[xt], writes=[junk, ss])
            op("scalar", lambda: nc.scalar.activation(out=std.t[:], in_=ss.t[:], func=AF.Sqrt, scale=1.0 / D,
                                                      bias=epsc.t[:, 0:1]), reads=[ss, epsc], writes=[std])
            op("vector", lambda: nc.vector.reciprocal(out=rstd.t[:], in_=std.t[:]), reads=[std], writes=[rstd])
            op("vector", lambda: nc.vector.scalar_tensor_tensor(out=hf.t[:], in0=xt.t[:], scalar=rstd.t[:, 0:1],
                                                                in1=A.t[:], op0=ALU.mult, op1=ALU.mult),
               reads=[xt, rstd, A], writes=[hf])
            if hb is not None:
                op("gpsimd", lambda: nc.gpsimd.tensor_tensor(out=hb.t[:], in0=hf.t[:], in1=B.t[:], op=ALU.add),
                   reads=[hf, B], writes=[hb])

        with ExitStack() as es:
            gqk = tile(es, "gqk", [128, 2], F32)
            gqs = tile(es, "gqs", [128, 1], F32)
            blkdiag = tile(es, "blkdiag", [128, 128], F32)
            dma("sync", gqk.t[:], gqk_d, writes=[gqk])
            dma("sync", blkdiag.t[:], blkdiag_d, writes=[blkdiag])
            op("vector", lambda: nc.vector.tensor_scalar(out=gqs.t[:], in0=gqk.t[:, 0:1], scalar1=0.125, scalar2=None,
                                                         op0=ALU.mult), reads=[gqk], writes=[gqs])
            kmean = tile(es, "kmean", [128, 4, 16], F32)
            op("vector", lambda: nc.vector.memset(kmean.t[:], 0.0), writes=[kmean])
            xb_ = [tile(es, f"xt{i}", [128, D], F32) for i in range(2)]
            hf = tile(es, "hf", [128, D], F32)
            hb = [tile(es, f"hb{i}", [128, D], BF16) for i in range(2)]
            junk = tile(es, "junk", [128, D], BF16)
            ss = tile(es, "ss", [128, 1], F32)
            std = tile(es, "std", [128, 1], F32)
            rstd = tile(es, "rstd", [128, 1], F32)
            hT = [tile(es, f"hT{i}", [128, 8, 512], BF16) for i in range(2)]
            vat = [tile(es, f"vat{i}", [128, H, 65], BF16) for i in range(2)]
            vbt = [tile(es, f"vbt{i}", [128, 512], BF16) for i in range(2)]
            for v in vat:
                op("vector", lambda: nc.vector.memset(v.t[:], 1.0), writes=[v])
            sq = tile(es, "sq", [128, 512], F32)
            sq_ = [sq, tile(es, "sq1", [128, 512], F32)]
            stdt = tile(es, "stdt", [128, 512], F32)
            rstdt = tile(es, "rstdt", [128, 512], F32)
            qn = tile(es, "qn", [128, 512], F32)
            ob_ = [tile(es, f"ob{i}", [128, 512], BF16) for i in range(3)]
            km2 = tile(es, "km2", [128, 2], F32)
            gsb = tile(es, "gsb", [128, 4, H, 16], F32)
            top8 = tile(es, "top8", [128, 4, H, 8], F32)
            sel = tile(es, "sel", [128, 4, H, 16], F32)
            mtb = tile(es, "mtb", [128, 512], BF16)
            ptr = [ptile(es, f"ptr{i}", [128, 1024], BF16) for i in range(2)]
            pmm = [ptile(es, f"pmm{i}", [128, 512], F32) for i in range(3)]
            pms = ptile(es, "pms", [128, 512], F32)
            pg = ptile(es, "pg", [128, 512], F32)
            pmt = ptile(es, "pmt", [128, 512], F32)
            obi = [0]
            pmi = [0]

            def next_ob():
                obi[0] += 1
                return ob_[obi[0] % 3]

            def next_pmm():
                pmi[0] += 1
                return pmm[pmi[0] % 3]

            def fm_chunk(hTg, c0):
                P = next_pmm()

                def mm():
                    for kc in range(8):
                        i = nc.tensor.matmul(P.t[:], lhsT=win.t[:, kc, c0:c0 + 128], rhs=hTg.t[:, kc, :],
                                             start=(kc == 0), stop=(kc == 7))
                    return i
                op("tensor", mm, reads=[win, hTg], writes=[P])
                return P

            def qknorm(P, gcol):
                op("scalar", lambda: nc.scalar.activation(out=sq.t[:], in_=P.t[:], func=AF.Square), reads=[P], writes=[sq])
                op("tensor", lambda: nc.tensor.matmul(pms.t[:], lhsT=blkdiag.t[:], rhs=sq.t[:], start=True, stop=True),
                   reads=[blkdiag, sq], writes=[pms])
                op("scalar", lambda: nc.scalar.activation(out=stdt.t[:], in_=pms.t[:], func=AF.Sqrt, bias=epsc.t[:, 0:1]),
                   reads=[pms, epsc], writes=[stdt])
                op("vector", lambda: nc.vector.reciprocal(out=rstdt.t[:], in_=stdt.t[:]), reads=[stdt], writes=[rstdt])
                op("vector", lambda: nc.vector.scalar_tensor_tensor(out=qn.t[:], in0=P.t[:], scalar=gcol, in1=rstdt.t[:],
                                                                    op0=ALU.mult, op1=ALU.mult),
                   reads=[P, rstdt, gqk, gqs], writes=[qn])

            def front_a(g, tt):
                k = g * 4 + tt
                tok0 = k * 128
                xt = xb_[k % 2]
                hbk = hb[k % 2]
                dma("sync", xt.t[:], x_d[tok0:tok0 + 128, :], writes=[xt])
                norm_mod(xt, A1, B1, hf, hbk, junk, ss, std, rstd)

            def front_b(g, tt):
                hTg = hT[g % 2]
                k = g * 4 + tt
                hbk = hb[k % 2]
                PT = ptr[k % 2]

                def tr():
                    for kc in range(8):
                        i = nc.tensor.transpose(out=PT.t[:, kc * 128:(kc + 1) * 128],
                                                in_=hbk.t[:, kc * 128:(kc + 1) * 128], identity=identb.t[:])
                    return i
                op("tensor", tr, reads=[hbk, identb], writes=[PT])
                op("vector", lambda: nc.vector.tensor_copy(out=hTg.t[:, :, tt * 128:(tt + 1) * 128],
                                                           in_=PT.t[:].rearrange("p (kc t) -> p kc t", kc=8)),
                   reads=[PT], writes=[hTg])

            def back(g):
                hTg = hT[g % 2]
                for tt in range(4):
                    k = g * 4 + tt
                    tok0 = k * 128
                    for (c0, isA) in ((1024, True), (2560, False)):
                        P = next_pmm()

                        def mm():
                            for kc in range(8):
                                i = nc.tensor.matmul(P.t[:], lhsT=hTg.t[:, kc, tt * 128:(tt + 1) * 128],
                                                     rhs=win.t[:, kc, c0:c0 + 512], start=(kc == 0), stop=(kc == 7))
                            return i
                        op("tensor", mm, reads=[win, hTg], writes=[P])
                        if isA:
                            V = vat[k % 2]
                            op("vector", lambda: nc.vector.tensor_copy(out=V.t[:, :, 0:64],
                                                                       in_=P.t[:].rearrange("p (h d) -> p h d", h=H)),
                               reads=[P], writes=[V])
                            dma("gpsimd", va_s[tok0:tok0 + 128, :], V.t[:].rearrange("p h d -> p (h d)"), reads=[V],
                                writes=[dB["va"]])
                        else:
                            V = vbt[k % 2]
                            op("scalar", lambda: nc.scalar.activation(out=V.t[:], in_=P.t[:], func=AF.Copy), reads=[P],
                               writes=[V])
                            dma("gpsimd", vb_s[tok0:tok0 + 128, :], V.t[:], reads=[V], writes=[dB["vb"]])
                    yield
                chunks = [("k", j) for j in range(4)] + [("q", j) for j in range(4)]
                held = {}

                def stA(i):
                    kind, j = chunks[i]
                    P = fm_chunk(hTg, (512 if kind == "k" else 0) + j * 128)
                    SQ = sq_[i % 2]
                    op("scalar", lambda: nc.scalar.activation(out=SQ.t[:], in_=P.t[:], func=AF.Square), reads=[P], writes=[SQ])
                    held[i] = (P, SQ)

                def stB(i):
                    kind, j = chunks[i]
                    P, SQ = held.pop(i)
                    gcol = gqk.t[:, 1:2] if kind == "k" else gqs.t[:, 0:1]
                    op("tensor", lambda: nc.tensor.matmul(pms.t[:], lhsT=blkdiag.t[:], rhs=SQ.t[:], start=True, stop=True),
                       reads=[blkdiag, SQ], writes=[pms])
                    op("scalar", lambda: nc.scalar.activation(out=stdt.t[:], in_=pms.t[:], func=AF.Sqrt, bias=epsc.t[:, 0:1]),
                       reads=[pms, epsc], writes=[stdt])
                    op("vector", lambda: nc.vector.reciprocal(out=rstdt.t[:], in_=stdt.t[:]), reads=[stdt], writes=[rstdt])
                    op("vector", lambda: nc.vector.scalar_tensor_tensor(out=qn.t[:], in0=P.t[:], scalar=gcol, in1=rstdt.t[:],
                                                                        op0=ALU.mult, op1=ALU.mult),
                       reads=[P, rstdt, gqk, gqs], writes=[qn])
                    if kind == "k":
                        op("vector", lambda: nc.vector.tensor_reduce(out=km2.t[:], in_=qn.t[:].rearrange("p (b t) -> p b t", b=2),
                                                                     axis=AX.X, op=ALU.add), reads=[qn], writes=[km2])
                        op("vector", lambda: nc.vector.tensor_scalar(out=kmean.t[:, j, 2 * g:2 * g + 2], in0=km2.t[:],
                                                                     scalar1=1.0 / 256, scalar2=None, op0=ALU.mult),
                           reads=[km2], writes=[kmean])
                        dst, nm = kaT_s, "kaT"
                    else:
                        def gm_():
                            for hh in range(2):
                                for tt in range(4):
                                    h = 2 * j + hh
                                    i_ = nc.tensor.matmul(pg.t[:, (tt * H + h) * 16:(tt * H + h + 1) * 16],
                                                          lhsT=qn.t[hh * 64:(hh + 1) * 64, tt * 128:(tt + 1) * 128],
                                                          rhs=kmean.t[hh * 64:(hh + 1) * 64, j, :], start=True, stop=True)
                            return i_
                        op("tensor", gm_, reads=[qn, kmean], writes=[pg])
                        dst, nm = qaT_s, "qaT"
                    O = next_ob()
                    op("gpsimd", lambda: nc.gpsimd.tensor_copy(out=O.t[:], in_=qn.t[:]), reads=[qn], writes=[O])
                    dma("scalar", dst[j * 128:(j + 1) * 128, g * 512:(g + 1) * 512], O.t[:], reads=[O], writes=[dB[nm]])

                stA(0)
                for i in range(8):
                    if i + 1 < 8:
                        stA(i + 1)
                    stB(i)
                    yield
                op("scalar", lambda: nc.scalar.activation(out=gsb.t[:].rearrange("p a b c -> p (a b c)"), in_=pg.t[:],
                                                          func=AF.Copy), reads=[pg], writes=[gsb])
                for tt in range(4):
                    own = 2 * g + tt // 2
                    op("vector", lambda: nc.vector.memset(gsb.t[:, tt, :, own:16], -1e30), writes=[gsb])
                for tt in range(4):
                    for h in range(H):
                        op("vector", lambda: nc.vector.max(out=top8.t[:, tt, h, :], in_=gsb.t[:, tt, h, :]), reads=[gsb],
                           writes=[top8])
                op("vector", lambda: nc.vector.tensor_tensor(out=sel.t[:], in0=gsb.t[:],
                                                             in1=top8.t[:, :, :, 2:3].to_broadcast([128, 4, H, 16]),
                                                             op=ALU.is_ge), reads=[gsb, top8], writes=[sel])
                for tt in range(4):
                    own = 2 * g + tt // 2
                    op("vector", lambda: nc.vector.memset(sel.t[:, tt, :, own:own + 1], 1.0), writes=[sel])
                    if own + 1 < 16:
                        op("vector", lambda: nc.vector.memset(sel.t[:, tt, :, own + 1:16], 0.0), writes=[sel])
                op("vector", lambda: nc.vector.tensor_scalar(out=sel.t[:], in0=sel.t[:], scalar1=1.0, scalar2=BIG,
                                                             op0=ALU.subtract, op1=ALU.mult), reads=[sel], writes=[sel])

                def trm():
                    for tt in range(4):
                        i = nc.tensor.transpose(out=pmt.t[:, tt * 128:(tt + 1) * 128],
                                                in_=sel.t[:, tt, :, :].rearrange("p a b -> p (a b)"), identity=identf.t[:])
                    return i
                op("tensor", trm, reads=[sel, identf], writes=[pmt])
                op("scalar", lambda: nc.scalar.activation(out=mtb.t[:], in_=pmt.t[:], func=AF.Copy), reads=[pmt], writes=[mtb])
                dma("scalar", mT_s[:, g * 512:(g + 1) * 512], mtb.t[:], reads=[mtb], writes=[dB["mT"]])
                yield
                for j in range(4):
                    P = fm_chunk(hTg, 1536 + j * 128)
                    O = next_ob()
                    op("scalar", lambda: nc.scalar.activation(out=O.t[:], in_=P.t[:], func=AF.Copy, scale=0.125),
                       reads=[P], writes=[O])
                    dma("scalar", qbT_s[j * 128:(j + 1) * 128, g * 512:(g + 1) * 512], O.t[:], reads=[O], writes=[dB["qbT"]])
                    yield
                for j in range(4):
                    P = fm_chunk(hTg, 2048 + j * 128)
                    O = next_ob()
                    op("vector", lambda: nc.vector.tensor_copy(out=O.t[:], in_=P.t[:]), reads=[P], writes=[O])
                    dma("scalar", kbT_s[j * 128:(j + 1) * 128, g * 512:(g + 1) * 512], O.t[:], reads=[O], writes=[dB["kbT"]])
                    yield
                for (c0, dst, nm) in ((3072, gaT_s, "gaT"), (4096, gbT_s, "gbT")):
                    for j in range(8):
                        P = fm_chunk(hTg, c0 + j * 128)
                        O = next_ob()
                        op("scalar", lambda: nc.scalar.activation(out=O.t[:], in_=P.t[:], func=AF.Sigmoid), reads=[P],
                           writes=[O])
                        dma("scalar", dst[j * 128:(j + 1) * 128, g * 512:(g + 1) * 512], O.t[:], reads=[O], writes=[dB[nm]])
                        yield
            for tt in range(4):
                front_a(0, tt)
                front_b(0, tt)
            PA = {3: 0, 11: 1, 19: 2, 27: 3}
            PB = {8: 0, 16: 1, 24: 2, 32: 3}
            for g in range(NG):
                n_y = 0
                for _ in back(g):
                    n_y += 1
                    if g + 1 < NG:
                        if n_y in PA:
                            front_a(g + 1, PA[n_y])
                        if n_y in PB:
                            front_b(g + 1, PB[n_y])
                assert n_y >= 32, n_y
            T.barrier()
        es_win.close()
        if STOP_AFTER <= 1:
            T.barrier()
            return nc

        with ExitStack() as es:
            vaug = tile(es, "vaug", [128, 32, H * 65], BF16)
            dma("sync", vaug.t[:], va_s.rearrange("(kb p) c -> p kb c", p=128), reads=[dB["va"]], writes=[vaug])
            zt = tile(es, "zt", [128, 8 * D], BF16)
            op("gpsimd", lambda: nc.gpsimd.memset(zt.t[:], 0.0), writes=[zt])
            xgv = xg_s.rearrange("(p r) d -> p r d", p=128)
            for zi in range(NROWS // 128 // 8):
                dma("scalar", xgv[:, zi * 8:(zi + 1) * 8, :], zt.t[:].rearrange("p (r d) -> p r d", r=8), reads=[zt],
                    writes=[dB["xg"]])
            b31 = tile(es, "b31", [128, H], F32)
            dma("sync", b31.t[:], bias31_d, writes=[b31])
            onesf = tile(es, "onesf", [128, 128], F32)
            dma("sync", onesf.t[:], onesf_d, writes=[onesf])
            qT = [tile(es, f"qT{i}", [80, S], BF16) for i in range(2)]
            kT = [tile(es, f"kT{i}", [80, S], BF16) for i in range(2)]
            for kt_ in kT:
                dma("sync", kt_.t[64:80, :], blkind_d, writes=[kt_])
            bt = [tile(es, f"bt{i}", [128, NOFF * 512], F32) for i in range(2)]
            NTMP, NPB, NPS = 3, 5, 4
            tmp = [tile(es, f"tmp{i}", [128, 512], F32) for i in range(NTMP)]
            pb = [tile(es, f"pb{i}", [128, 512], BF16) for i in range(NPB)]
            osb = [tile(es, f"osb{i}", [64, 512], F32) for i in range(2)]
            rden = [tile(es, f"rden{i}", [128, 512], F32) for i in range(2)]
            onb = [tile(es, f"onb{i}", [64, 512], BF16) for i in range(2)]
            pS = [ptile(es, f"pS{i}", [128, 512], F32) for i in range(NPS)]
            pacc = [ptile(es, f"pacc{i}", [128, 512], F32) for i in range(2)]
            pbc = ptile(es, "pbc", [128, 512], F32)

            def load_head(h):
                i = h % 2
                dma("sync", qT[i].t[0:64, :], qaT_s[h * 64:(h + 1) * 64, :], reads=[dB["qaT"]], writes=[qT[i]])
                dma("sync", qT[i].t[64:80, :], mT_s[h * 16:(h + 1) * 16, :], reads=[dB["mT"]], writes=[qT[i]])
                dma("sync", kT[i].t[0:64, :], kaT_s[h * 64:(h + 1) * 64, :], reads=[dB["kaT"]], writes=[kT[i]])
                dma("sync", bt[i].t[:], biast_d[h], writes=[bt[i]])
            jobs = [(h, qt, kb) for h in range(H) for qt in range(8) for kb in range(4 * qt + 4)]
            NJ = len(jobs)
            LAG = 3
            load_head(0)
            fin2 = {}

            def stage_front(t):
                h, qt, kb = jobs[t]
                if qt == 0 and kb == 0 and h + 1 < H:
                    load_head(h + 1)
                Q, K_, BT = qT[h % 2], kT[h % 2], bt[h % 2]
                Sx = pS[t % NPS]
                op("tensor", lambda: nc.tensor.matmul(Sx.t[:], lhsT=K_.t[:, kb * 128:(kb + 1) * 128],
                                                      rhs=Q.t[:, qt * 512:(qt + 1) * 512], start=True, stop=True),
                   reads=[K_, Q], writes=[Sx])
                delta = qt * 512 - kb * 128
                P = pb[t % NPB]
                if delta < 917:
                    oi = (delta + 384) // 128
                    tm = tmp[t % NTMP]
                    op("vector", lambda: nc.vector.tensor_tensor(out=tm.t[:], in0=Sx.t[:],
                                                                 in1=BT.t[:, oi * 512:(oi + 1) * 512], op=ALU.add),
                       reads=[Sx, BT], writes=[tm])
                    op("scalar", lambda: nc.scalar.activation(out=P.t[:], in_=tm.t[:], func=AF.Exp), reads=[tm], writes=[P])
                else:
                    op("scalar", lambda: nc.scalar.activation(out=P.t[:], in_=Sx.t[:], func=AF.Exp, bias=b31.t[:, h:h + 1]),
                       reads=[Sx, b31], writes=[P])

            def stage_back(t, step):
                h, qt, kb = jobs[t]
                nkb = 4 * qt + 4
                qi = h * 8 + qt
                ACC = pacc[qi % 2]
                P = pb[t % NPB]
                op("tensor", lambda: nc.tensor.matmul(ACC.t[0:65, :], lhsT=vaug.t[:, kb, h * 65:(h + 1) * 65], rhs=P.t[:],
                                                      start=(kb == 0), stop=(kb == nkb - 1)), reads=[vaug, P], writes=[ACC])
                if kb == nkb - 1:
                    OS, RD = osb[qi % 2], rden[qi % 2]
                    op("scalar", lambda: nc.scalar.activation(out=OS.t[:], in_=ACC.t[0:64, :], func=AF.Copy), reads=[ACC],
                       writes=[OS])
                    op("vector", lambda: nc.vector.reciprocal(out=RD.t[64:65, :], in_=ACC.t[64:65, :]), reads=[ACC], writes=[RD])

                    def f2():
                        op("tensor", lambda: nc.tensor.matmul(pbc.t[0:64, :], lhsT=onesf.t[64:65, 0:64], rhs=RD.t[64:65, :],
                                                              start=True, stop=True), reads=[onesf, RD], writes=[pbc])
                        ON = onb[qi % 2]
                        op("vector", lambda: nc.vector.tensor_tensor(out=ON.t[:], in0=OS.t[:], in1=pbc.t[0:64, :], op=ALU.mult),
                           reads=[OS, pbc], writes=[ON])
                        dma("gpsimd", oaT_s[h * 64:(h + 1) * 64, qt * 512:(qt + 1) * 512], ON.t[:], reads=[ON],
                            writes=[dB["oaT"]])
                    fin2.setdefault(step + 3, []).append(f2)

            for step in range(NJ + LAG + 5):
                if step < NJ:
                    stage_front(step)
                if 0 <= step - LAG < NJ:
                    stage_back(step - LAG, step)
                for f in fin2.pop(step, []):
                    f()
            assert not fin2
            T.barrier()
        if STOP_AFTER <= 2:
            T.barrier()
            return nc

        with ExitStack() as es:
            dvh = tile(es, "dvh", [128, 32, 512], BF16)
            dvl = tile(es, "dvl", [128, 32, 512], BF16)
            vbv = vb_s.rearrange("(kb p) c -> p kb c", p=128)
            antiI = tile(es, "antiI", [128, 128], BF16)
            nantiI = tile(es, "nantiI", [128, 128], BF16)
            dma("sync", antiI.t[:], antii_d, writes=[antiI])
            op("vector", lambda: nc.vector.tensor_scalar(out=nantiI.t[:], in0=antiI.t[:], scalar1=-1.0, scalar2=None, op0=ALU.mult),
               reads=[antiI], writes=[nantiI])
            NOM, NPBUF, NXT, NZ, NPT = 3, 4, 4, 3, 2
            pz = [ptile(es, f"pz{i}", [128, 512], F32) for i in range(NZ)]
            ptt = [ptile(es, f"ptt{i}", [128, 512], F32) for i in range(NPT)]
            pacc = [ptile(es, f"spacc{i}", [128, 512], F32) for i in range(2)]
            ptot = ptile(es, "ptot", [128, 512], F32)
            with ExitStack() as es_in:
                vb = tile(es_in, "vb", [128, 32, 512], BF16)
                vb1 = tile(es_in, "vb1", [128, 32, 512], BF16)
                dma("sync", vb.t[:], vbv, reads=[dB["vb"]], writes=[vb])
                op("gpsimd", lambda: nc.gpsimd.memset(vb1.t[:], 0.0), writes=[vb1])
                dma("sync", vb1.t[:, 0:31, :], vb_s[1:1 + 31 * 128, :].rearrange("(kb p) c -> p kb c", p=128), reads=[dB["vb"]],
                    writes=[vb1])
                dma("sync", vb1.t[0:127, 31, :], vb_s[31 * 128 + 1:S, :], reads=[dB["vb"]], writes=[vb1])
                for kb in range(32):
                    Pz = pz[kb % NZ]

                    def mmr():
                        nc.tensor.matmul(Pz.t[:], lhsT=antiI.t[:], rhs=vb.t[:, kb, :], start=True, stop=False)
                        return nc.tensor.matmul(Pz.t[:], lhsT=nantiI.t[:], rhs=vb1.t[:, kb, :], start=False, stop=True)
                    op("tensor", mmr, reads=[antiI, nantiI, vb, vb1], writes=[Pz])
                    op("scalar", lambda: nc.scalar.activation(out=dvh.t[:, kb, :], in_=Pz.t[:], func=AF.Copy), reads=[Pz],
                       writes=[dvh])
                    op("vector", lambda: nc.vector.tensor_tensor(out=dvl.t[:, kb, :], in0=Pz.t[:], in1=dvh.t[:, kb, :],
                                                                 op=ALU.subtract), reads=[Pz, dvh], writes=[dvl])
                T.barrier()
            diagm = tile(es, "diagm", [128, 128], F32)
            dma("sync", diagm.t[:], diagm_d, writes=[diagm])
            zer = tile(es, "zer", [128, 512], F32)
            op("vector", lambda: nc.vector.memset(zer.t[:], 0.0), writes=[zer])
            onesrow = tile(es, "onesrow", [1, 128], BF16)
            op("vector", lambda: nc.vector.memset(onesrow.t[:], 1.0), writes=[onesrow])
            qT = [tile(es, f"sqT{i}", [64, S], BF16) for i in range(2)]
            kT = [tile(es, f"skT{i}", [64, S], BF16) for i in range(2)]
            vfirst_ = [tile(es, f"vfirst{i}", [1, 32, 64], BF16) for i in range(3)]
            oms = [tile(es, f"oms{i}", [128, 516], F32) for i in range(NOM)]
            pbx = [tile(es, f"pbx{i}", [128, 512], F32) for i in range(NPBUF)]
            pxt = [tile(es, f"pxt{i}", [128, 512], BF16) for i in range(NXT)]
            tot = [tile(es, f"tot{i}", [128, 1], F32) for i in range(2)]
            dgt = [tile(es, f"dgt{i}", [128, 128], BF16) for i in range(8)]
            oacc = [tile(es, f"oacc{i}", [64, 128], F32) for i in range(2)]
            zcol = tile(es, "zcol", [128, 128], BF16)
            op("vector", lambda: nc.vector.memset(zcol.t[:], 0.0), writes=[zcol])
            v0b_ = [tile(es, f"v0b{i}", [128, 64], BF16) for i in range(3)]
            onb = [tile(es, f"sonb{i}", [64, 512], BF16) for i in range(2)]

            def load_head(h):
                i = h % 2
                dma("sync", qT[i].t[:], qbT_s[h * 64:(h + 1) * 64, :], reads=[dB["qbT"]], writes=[qT[i]])
                dma("sync", kT[i].t[:], kbT_s[h * 64:(h + 1) * 64, :], reads=[dB["kbT"]], writes=[kT[i]])
                i3 = h % 3
                dma("sync", vfirst_[i3].t[:], vbv[0:1, :, h * 64:(h + 1) * 64], reads=[dB["vb"]], writes=[vfirst_[i3]])
                dma("sync", v0b_[i3].t[:], vb_s[0:1, h * 64:(h + 1) * 64].partition_broadcast(128), reads=[dB["vb"]],
                    writes=[v0b_[i3]])
                op("vector", lambda: nc.vector.tensor_scalar(out=v0b_[i3].t[:], in0=v0b_[i3].t[:], scalar1=-1.0, scalar2=None,
                                                             op0=ALU.mult), reads=[v0b_[i3]], writes=[v0b_[i3]])
            jobs = []
            for h in range(H):
                for qi in range(32):
                    q0 = qi * 128
                    nch = -(-(q0 + 128) // 512)
                    for c in range(nch):
                        k_hi = q0 + 128 - 512 * c
                        k_lo = max(0, k_hi - 512)
                        jobs.append((h, qi, c, nch, k_lo, k_hi))
            NJ = len(jobs)
            load_head(0)

            def stF(t):
                h, qi, c, nch, k_lo, k_hi = jobs[t]
                if qi == 0 and c == 0 and h + 1 < H:
                    load_head(h + 1)
                w = k_hi - k_lo
                q0 = qi * 128
                Q, K_ = qT[h % 2], kT[h % 2]
                Z, OMS, PBX = pz[t % NZ], oms[t % NOM], pbx[t % NPBUF]
                op("tensor", lambda: nc.tensor.matmul(Z.t[:, 0:w], lhsT=Q.t[:, q0:q0 + 128], rhs=K_.t[:, k_lo:k_hi],
                                                      start=True, stop=True), reads=[Q, K_], writes=[Z])
                op("scalar", lambda: nc.scalar.activation(out=OMS.t[:, w:0:-1], in_=Z.t[:, 0:w], func=AF.Sigmoid, scale=-1.0),
                   reads=[Z], writes=[OMS])
                if c < nch - 1:
                    OMSn = oms[(t + 1) % NOM]
                    op("scalar", lambda: nc.scalar.activation(out=OMSn.t[:, 0:1], in_=Z.t[:, 0:1], func=AF.Sigmoid, scale=-1.0),
                       reads=[Z], writes=[OMSn])
                if c == 0:
                    op("vector", lambda: nc.vector.tensor_tensor(out=OMS.t[:, 1:129], in0=OMS.t[:, 1:129], in1=diagm.t[:],
                                                                 op=ALU.max), reads=[OMS, diagm], writes=[OMS])
                    op("vector", lambda: nc.vector.memset(OMS.t[:, 0:1], 1.0), writes=[OMS])
                    init = 1.0
                    rd = [OMS, zer]
                else:
                    PBp = pbx[(t - 1) % NPBUF]
                    wp = jobs[t - 1][5] - jobs[t - 1][4]
                    init = PBp.t[:, wp - 1:wp]
                    rd = [OMS, zer, PBp]
                op("vector", lambda: nc.vector.tensor_tensor_scan(out=PBX.t[:, 0:w], data0=OMS.t[:, 0:w], data1=zer.t[:, 0:w],
                                                                  initial=init, op0=ALU.mult, op1=ALU.add),
                   reads=rd, writes=[PBX])
                if c == nch - 1:
                    TT = tot[(h * 32 + qi) % 2]
                    op("vector", lambda: nc.vector.tensor_tensor(out=TT.t[:], in0=PBX.t[:, w - 1:w], in1=OMS.t[:, w:w + 1],
                                                                 op=ALU.mult), reads=[PBX, OMS], writes=[TT])
                    DG = dgt[(h * 32 + qi) % 8]
                    op("vector", lambda: nc.vector.tensor_scalar(out=DG.t[:], in0=identf.t[:], scalar1=TT.t[:, 0:1], scalar2=None,
                                                                 op0=ALU.mult), reads=[identf, TT], writes=[DG])
                    if DEBUG and t == 0:
                        dma("sync", dbgf_s[:, 0:2], OMS.t[:, 127:129], reads=[OMS])
                        tt2 = tile(es, "tt2", [128, 2], F32)
                        op("vector", lambda: nc.vector.tensor_copy(out=tt2.t[:], in_=TT.t[:, 0:1].to_broadcast([128, 2])), reads=[TT], writes=[tt2])
                        dma("sync", dbgf_s[:, 2:4], tt2.t[:], reads=[tt2])
                        dma("sync", dbgf_s[:, 1:517], OMS.t[:, 0:516], reads=[OMS])
                        dma("sync", dbgf_s[:, 520:1032], PBX.t[:, :], reads=[PBX])
                        dma("sync", dbgb_s[:, 0:128], DG.t[:], reads=[DG])
                        dma("sync", dbgb_s[:, 128:192], v0b_[0].t[:], reads=[v0b_[0]])

            def stT(t):
                h, qi, c, nch, k_lo, k_hi = jobs[t]
                w = k_hi - k_lo
                PBX, PT, PXT = pbx[t % NPBUF], ptt[t % NPT], pxt[t % NXT]

                def tr():
                    for b in range(w // 128):
                        i = nc.tensor.transpose(out=PT.t[:, b * 128:(b + 1) * 128], in_=PBX.t[:, b * 128:(b + 1) * 128],
                                                identity=identf.t[:])
                    return i
                op("tensor", tr, reads=[PBX, identf], writes=[PT])
                op("scalar", lambda: nc.scalar.activation(out=PXT.t[:, 0:w], in_=PT.t[:, 0:w], func=AF.Copy), reads=[PT],
                   writes=[PXT])

            def stP(t):
                h, qi, c, nch, k_lo, k_hi = jobs[t]
                w = k_hi - k_lo
                PXT, ACC = pxt[t % NXT], pacc[(h * 32 + qi) % 2]
                nb = w // 128
                hs = slice(h * 64, (h + 1) * 64)
                vfirst = vfirst_[h % 3]
                last = (c == nch - 1)

                def mm():
                    for b in range(nb):
                        kb = (k_hi - 128 * (b + 1)) // 128
                        nc.tensor.matmul(ACC.t[0:64, 0:128], lhsT=dvh.t[:, kb, hs], rhs=PXT.t[:, b * 128:(b + 1) * 128],
                                         start=(c == 0 and b == 0), stop=False)
                        i = nc.tensor.matmul(ACC.t[0:64, 0:128], lhsT=dvl.t[:, kb, hs], rhs=PXT.t[:, b * 128:(b + 1) * 128],
                                             start=False, stop=False)
                    return i
                op("tensor", mm, reads=[dvh, dvl, PXT], writes=[ACC])
                if last:
                    DG, V0B = dgt[(h * 32 + qi) % 8], v0b_[h % 3]

                    def mm2():
                        if qi < 31:
                            return nc.tensor.matmul(ACC.t[0:64, 0:128], lhsT=vfirst.t[0:1, qi + 1, :], rhs=onesrow.t[0:1, :],
                                                    start=False, stop=True)
                        return nc.tensor.matmul(ACC.t[0:64, 0:128], lhsT=dvl.t[:, 0, hs], rhs=zcol.t[:, :], start=False, stop=True)
                    op("tensor", mm2, reads=[vfirst, onesrow, zcol, dvl], writes=[ACC])
                    op("tensor", lambda: nc.tensor.matmul(ptot.t[0:64, 0:128], lhsT=V0B.t[:, :], rhs=DG.t[:, :], start=True, stop=True),
                       reads=[DG, V0B], writes=[ptot])
                    OA = oacc[(h * 32 + qi) % 2]
                    op("scalar", lambda: nc.scalar.activation(out=OA.t[:], in_=ACC.t[0:64, 0:128], func=AF.Copy), reads=[ACC], writes=[OA])
                    ON = onb[((h * 32 + qi) // 4) % 2]
                    op("vector", lambda: nc.vector.tensor_tensor(out=ON.t[:, (qi % 4) * 128:(qi % 4 + 1) * 128], in0=OA.t[:],
                                                                 in1=ptot.t[0:64, 0:128], op=ALU.add), reads=[OA, ptot], writes=[ON])
                    if qi % 4 == 3:
                        g4 = qi // 4
                        dma("gpsimd", obT_s[h * 64:(h + 1) * 64, g4 * 512:(g4 + 1) * 512], ON.t[:], reads=[ON], writes=[dB["obT"]])

            L1, L2 = 3, 5
            for step in range(NJ + L2 + 1):
                if step < NJ:
                    stF(step)
                if 0 <= step - L1 < NJ:
                    stT(step - L1)
                if 0 <= step - L2 < NJ:
                    stP(step - L2)
            T.barrier()
        if STOP_AFTER <= 3:
            T.barrier()
            return nc

        with ExitStack() as es:
            wbra = tile(es, "wbra", [128, 4, D], BF16)
            wbrb = tile(es, "wbrb", [128, 4, D], BF16)
            wout = tile(es, "wout", [128, 8, D], BF16)
            wr = tile(es, "wr", [128, 8, 36], F32)
            brb = tile(es, "brb", [128, 36], F32)
            ustrict = tile(es, "ustrict", [128, 128], F32)
            onesf = tile(es, "onesf3", [128, 128], F32)
            baserow = tile(es, "baserow", [128, NE], F32)
            selsum = tile(es, "selsum", [128, NE], F32)
            rank_all = tile(es, "rank_all", [128, 32, NE], F32)
            t1_all = tile(es, "t1_all", [128, 32, NE], F32)
            t2_all = tile(es, "t2_all", [128, 32, NE], F32)
            big = tile(es, "big", [128, NBLK * NE], F32)
            big2 = tile(es, "big2", [128, NBLK * NE], F32)
            dma("gpsimd", wbra.t[:], wbra_d.rearrange("(kc p) c -> p kc c", p=128), writes=[wbra])
            dma("gpsimd", wbrb.t[:], wbrb_d.rearrange("(kc p) c -> p kc c", p=128), writes=[wbrb])
            dma("gpsimd", wout.t[:], wout_d.rearrange("(kc p) c -> p kc c", p=128), writes=[wout])
            dma("sync", wr.t[:], wr_d.rearrange("(kc p) c -> p kc c", p=128), writes=[wr])
            dma("sync", brb.t[:], br_d[0:1, :].partition_broadcast(128), writes=[brb])
            dma("sync", ustrict.t[:], ustrict_d, writes=[ustrict])
            dma("sync", onesf.t[:], onesf_d, writes=[onesf])
            op("vector", lambda: nc.vector.memset(selsum.t[:], 0.0), writes=[selsum])
            oat = [tile(es, f"oat{i}", [128, 4, 512], BF16) for i in range(2)]
            obt = [tile(es, f"obt{i}", [128, 4, 512], BF16) for i in range(2)]
            gat = [tile(es, f"gat{i}", [128, 8, 512], BF16) for i in range(2)]
            gbt = [tile(es, f"gbt{i}", [128, 8, 512], BF16) for i in range(2)]
            m1 = tile(es, "m1", [128, 512], F32)
            m2 = tile(es, "m2", [128, 512], F32)
            mg = tile(es, "mg", [128, 8, 512], BF16)
            xt_ = [tile(es, f"x3t{i}", [128, D], F32) for i in range(2)]
            x1t = [tile(es, f"x1t{i}", [128, D], F32) for i in range(2)]
            tmpx = tile(es, "tmpx", [128, D], F32)
            hf = tile(es, "h2f0", [128, D], F32)
            h2f = tile(es, "h2f", [128, D], F32)
            h2b = [tile(es, f"h2b{i}", [128, D], BF16) for i in range(2)]
            junk = tile(es, "junk3", [128, D], BF16)
            ss = tile(es, "ss3", [128, 1], F32)
            std = tile(es, "std3", [128, 1], F32)
            rstd = tile(es, "rstd3", [128, 1], F32)
            h2T = tile(es, "h2T", [128, 8, 128], F32)
            lg_ = [tile(es, f"lg{i}", [128, 36], F32) for i in range(2)]
            sm = {n: tile(es, "sm_" + n, [128, w], F32) for n, w in
                  [("gmax", 1), ("ngmax", 1), ("gexp", 4), ("gsum", 1), ("gw", 1), ("goh", 4), ("pen", 4), ("em", 32),
                   ("t8", 8), ("sel", 32), ("t1", 32), ("t2", 32), ("dd", 1), ("ex", 1), ("den", 1), ("rr", 1),
                   ("dest", 32), ("jk", 32), ("dAf", 1), ("dBf", 1)]}
            pA = ptile(es, "p3A", [128, 512], F32)
            pB = ptile(es, "p3B", [128, 512], F32)
            py = [ptile(es, f"p3y{i}", [128, 512], F32) for i in range(2)]
            ptf = [ptile(es, f"p3t{i}", [128, 512], F32) for i in range(2)]
            plg = ptile(es, "p3lg", [128, 512], F32)
            prk = ptile(es, "p3rk", [128, 512], F32)

            def load_group(g):
                i = g % 2
                sl = slice(g * 512, (g + 1) * 512)
                dma("sync", oat[i].t[:], oaT_s.rearrange("(kc p) t -> p kc t", p=128)[:, :, sl], reads=[dB["oaT"]], writes=[oat[i]])
                dma("sync", obt[i].t[:], obT_s.rearrange("(kc p) t -> p kc t", p=128)[:, :, sl], reads=[dB["obT"]], writes=[obt[i]])
                dma("sync", gat[i].t[:], gaT_s.rearrange("(kc p) t -> p kc t", p=128)[:, :, sl], reads=[dB["gaT"]], writes=[gat[i]])
                dma("sync", gbt[i].t[:], gbT_s.rearrange("(kc p) t -> p kc t", p=128)[:, :, sl], reads=[dB["gbT"]], writes=[gbt[i]])
            def tile_body(g, tt):
                k = g * 4 + tt
                tok0 = k * 128
                xt = xt_[k % 2]
                X1 = x1t[k % 2]
                H2B = h2b[k % 2]
                dma("sync", xt.t[:], x_d[tok0:tok0 + 128, :], writes=[xt])
                for half in range(2):
                    PY = py[half]

                    def mmy():
                        for kc in range(8):
                            i = nc.tensor.matmul(PY.t[:], lhsT=mg.t[:, kc, tt * 128:(tt + 1) * 128],
                                                 rhs=wout.t[:, kc, half * 512:(half + 1) * 512], start=(kc == 0), stop=(kc == 7))
                        return i
                    op("tensor", mmy, reads=[mg, wout], writes=[PY])
                    op("vector", lambda: nc.vector.tensor_tensor(out=tmpx.t[:, half * 512:(half + 1) * 512], in0=PY.t[:],
                                                                 in1=gtA.t[:, half * 512:(half + 1) * 512], op=ALU.mult),
                       reads=[PY, gtA], writes=[tmpx])
                op("gpsimd", lambda: nc.gpsimd.tensor_tensor(out=X1.t[:], in0=tmpx.t[:], in1=xt.t[:], op=ALU.add),
                   reads=[tmpx, xt], writes=[X1])
                dma("scalar", x1_s[tok0:tok0 + 128, :], X1.t[:], reads=[X1], writes=[dB["x1"]])
                norm_mod(X1, A2, B2, hf, None, junk, ss, std, rstd)
                op("vector", lambda: nc.vector.tensor_tensor(out=h2f.t[:], in0=hf.t[:], in1=B2.t[:], op=ALU.add),
                   reads=[hf, B2], writes=[h2f])
                op("gpsimd", lambda: nc.gpsimd.tensor_copy(out=H2B.t[:], in_=h2f.t[:]), reads=[h2f], writes=[H2B])
                for half in range(2):
                    PT = ptf[half]

                    def tr():
                        for c in range(4):
                            kc = half * 4 + c
                            i = nc.tensor.transpose(out=PT.t[:, c * 128:(c + 1) * 128], in_=h2f.t[:, kc * 128:(kc + 1) * 128],
                                                    identity=identf.t[:])
                        return i
                    op("tensor", tr, reads=[h2f, identf], writes=[PT])
                    op("scalar", lambda: nc.scalar.activation(out=h2T.t[:, half * 4:(half + 1) * 4, :],
                                                              in_=PT.t[:].rearrange("p (c t) -> p c t", c=4), func=AF.Copy),
                       reads=[PT], writes=[h2T])

                def mml():
                    for kc in range(8):
                        i = nc.tensor.matmul(plg.t[:, 0:36], lhsT=h2T.t[:, kc, :], rhs=wr.t[:, kc, :], start=(kc == 0),
                                             stop=(kc == 7))
                    return i
                op("tensor", mml, reads=[h2T, wr], writes=[plg])
                V = nc.vector
                s = sm
                lg = lg_[k % 2]
                op("vector", lambda: V.tensor_tensor(out=lg.t[:], in0=plg.t[:, 0:36], in1=brb.t[:], op=ALU.add),
                   reads=[plg, brb], writes=[lg])
                yield
                op("vector", lambda: V.tensor_reduce(out=s["gmax"].t[:], in_=lg.t[:, 0:4], axis=AX.X, op=ALU.max),
                   reads=[lg], writes=[s["gmax"]])
                op("vector", lambda: V.tensor_scalar(out=s["ngmax"].t[:], in0=s["gmax"].t[:], scalar1=-1.0, scalar2=None,
                                                     op0=ALU.mult), reads=[s["gmax"]], writes=[s["ngmax"]])
                op("scalar", lambda: nc.scalar.activation(out=s["gexp"].t[:], in_=lg.t[:, 0:4], func=AF.Exp,
                                                          bias=s["ngmax"].t[:, 0:1], accum_out=s["gsum"].t[:, 0:1]),
                   reads=[lg, s["ngmax"]], writes=[s["gexp"], s["gsum"]])
                op("vector", lambda: V.reciprocal(out=s["gw"].t[:], in_=s["gsum"].t[:]), reads=[s["gsum"]], writes=[s["gw"]])
                op("vector", lambda: V.tensor_scalar(out=s["goh"].t[:], in0=lg.t[:, 0:4], scalar1=s["gmax"].t[:, 0:1],
                                                     scalar2=None, op0=ALU.is_ge), reads=[lg, s["gmax"]], writes=[s["goh"]])
                op("vector", lambda: V.tensor_scalar(out=s["pen"].t[:], in0=s["goh"].t[:], scalar1=1.0, scalar2=1e30,
                                                     op0=ALU.subtract, op1=ALU.mult), reads=[s["goh"]], writes=[s["pen"]])
                op("vector", lambda: V.tensor_tensor(out=s["em"].t[:].rearrange("p (g e) -> p g e", g=4),
                                                     in0=lg.t[:, 4:36].rearrange("p (g e) -> p g e", g=4),
                                                     in1=s["pen"].t[:, :].unsqueeze(2).to_broadcast([128, 4, 8]), op=ALU.add),
                   reads=[lg, s["pen"]], writes=[s["em"]])
                op("vector", lambda: V.max(out=s["t8"].t[:], in_=s["em"].t[:]), reads=[s["em"]], writes=[s["t8"]])
                op("vector", lambda: V.tensor_scalar(out=s["sel"].t[:], in0=s["em"].t[:], scalar1=s["t8"].t[:, 1:2],
                                                     scalar2=None, op0=ALU.is_ge), reads=[s["em"], s["t8"]], writes=[s["sel"]])
                op("vector", lambda: V.tensor_scalar(out=s["t1"].t[:], in0=s["em"].t[:], scalar1=s["t8"].t[:, 0:1],
                                                     scalar2=None, op0=ALU.is_ge), reads=[s["em"], s["t8"]], writes=[s["t1"]])
                op("vector", lambda: V.tensor_tensor(out=s["t2"].t[:], in0=s["sel"].t[:], in1=s["t1"].t[:], op=ALU.subtract),
                   reads=[s["sel"], s["t1"]], writes=[s["t2"]])
                op("vector", lambda: V.tensor_tensor(out=s["dd"].t[:], in0=s["t8"].t[:, 1:2], in1=s["t8"].t[:, 0:1],
                                                     op=ALU.subtract), reads=[s["t8"]], writes=[s["dd"]])
                op("scalar", lambda: nc.scalar.activation(out=s["ex"].t[:], in_=s["dd"].t[:], func=AF.Exp), reads=[s["dd"]],
                   writes=[s["ex"]])
                op("vector", lambda: V.tensor_scalar(out=s["den"].t[:], in0=s["ex"].t[:], scalar1=1.0, scalar2=None,
                                                     op0=ALU.add), reads=[s["ex"]], writes=[s["den"]])
                op("vector", lambda: V.reciprocal(out=s["rr"].t[:], in_=s["den"].t[:]), reads=[s["den"]], writes=[s["rr"]])
                op("vector", lambda: V.tensor_tensor(out=wA_all.t[:, k:k + 1], in0=s["rr"].t[:], in1=s["gw"].t[:], op=ALU.mult),
                   reads=[s["rr"], s["gw"]], writes=[wA_all])
                op("vector", lambda: V.tensor_tensor(out=wB_all.t[:, k:k + 1], in0=s["gw"].t[:], in1=wA_all.t[:, k:k + 1],
                                                     op=ALU.subtract), reads=[s["gw"], wA_all], writes=[wB_all])

                def mmr():
                    nc.tensor.matmul(prk.t[:, 0:32], lhsT=ustrict.t[:], rhs=s["sel"].t[:], start=True, stop=False)
                    return nc.tensor.matmul(prk.t[:, 0:32], lhsT=onesf.t[:], rhs=selsum.t[:], start=False, stop=True)
                op("tensor", mmr, reads=[ustrict, onesf, s["sel"], selsum], writes=[prk])
                op("scalar", lambda: nc.scalar.activation(out=rank_all.t[:, k, :], in_=prk.t[:, 0:32], func=AF.Copy),
                   reads=[prk], writes=[rank_all])
                op("vector", lambda: V.tensor_tensor(out=selsum.t[:], in0=selsum.t[:], in1=s["sel"].t[:], op=ALU.add),
                   reads=[selsum, s["sel"]], writes=[selsum])
                op("vector", lambda: V.tensor_copy(out=t1_all.t[:, k, :], in_=s["t1"].t[:]), reads=[s["t1"]], writes=[t1_all])
                op("vector", lambda: V.tensor_copy(out=t2_all.t[:, k, :], in_=s["t2"].t[:]), reads=[s["t2"]], writes=[t2_all])
                dma("scalar", h2_s[tok0:tok0 + 128, :], H2B.t[:], reads=[H2B], writes=[dB["h2"]])

            pend_gen = []
            load_group(0)
            for g in range(NG):
                if g + 1 < NG:
                    load_group(g + 1)
                OA, OB, GA, GB = oat[g % 2], obt[g % 2], gat[g % 2], gbt[g % 2]
                for dc in range(8):
                    def mmA():
                        for kc in range(4):
                            i = nc.tensor.matmul(pA.t[:], lhsT=wbra.t[:, kc, dc * 128:(dc + 1) * 128], rhs=OA.t[:, kc, :],
                                                 start=(kc == 0), stop=(kc == 3))
                        return i

                    def mmB():
                        for kc in range(4):
                            i = nc.tensor.matmul(pB.t[:], lhsT=wbrb.t[:, kc, dc * 128:(dc + 1) * 128], rhs=OB.t[:, kc, :],
                                                 start=(kc == 0), stop=(kc == 3))
                        return i
                    op("tensor", mmA, reads=[wbra, OA], writes=[pA])
                    op("tensor", mmB, reads=[wbrb, OB], writes=[pB])
                    op("vector", lambda: nc.vector.tensor_tensor(out=m1.t[:], in0=pA.t[:], in1=GA.t[:, dc, :], op=ALU.mult),
                       reads=[pA, GA], writes=[m1])
                    op("vector", lambda: nc.vector.tensor_tensor(out=m2.t[:], in0=pB.t[:], in1=GB.t[:, dc, :], op=ALU.mult),
                       reads=[pB, GB], writes=[m2])
                    op("gpsimd", lambda: nc.gpsimd.tensor_tensor(out=mg.t[:, dc, :], in0=m1.t[:], in1=m2.t[:], op=ALU.add),
                       reads=[m1, m2], writes=[mg])
                for tt in range(4):
                    gen = tile_body(g, tt)
                    next(gen)
                    for pg_ in pend_gen:
                        for _ in pg_:
                            pass
                    pend_gen = [gen]
            for pg_ in pend_gen:
                for _ in pg_:
                    pass
            V = nc.vector
            op("tensor", lambda: nc.tensor.matmul(prk.t[:, 0:32], lhsT=onesf.t[:], rhs=selsum.t[:], start=True, stop=True),
               reads=[onesf, selsum], writes=[prk])
            cnt = tile(es, "cnt", [128, NE], F32)
            nblk = tile(es, "nblk", [128, NE], F32)
            pend = tile(es, "pend", [128, NE], F32)
            onesr = tile(es, "onesr", [128, NE], F32)
            blke = tile(es, "blke", [128, NBLK], F32)
            pcol = tile(es, "pcol", [128, 1], F32)
            dma("sync", pcol.t[:], pcol_d, writes=[pcol])
            op("vector", lambda: V.tensor_copy(out=cnt.t[:], in_=prk.t[:, 0:32]), reads=[prk], writes=[cnt])
            op("vector", lambda: V.memset(onesr.t[:], 1.0), writes=[onesr])
            dma("sync", big.t[:, 0:NE * 32], thr_d, writes=[big])
            op("vector", lambda: V.tensor_tensor(out=big2.t[:, 0:NE * 32].rearrange("p (e m) -> p e m", e=NE),
                                                 in0=cnt.t[:, :].unsqueeze(2).to_broadcast([128, NE, 32]),
                                                 in1=big.t[:, 0:NE * 32].rearrange("p (e m) -> p e m", e=NE), op=ALU.is_gt),
               reads=[cnt, big], writes=[big2])
            op("vector", lambda: V.tensor_reduce(out=nblk.t[:], in_=big2.t[:, 0:NE * 32].rearrange("p (e m) -> p e m", e=NE),
                                                 axis=AX.X, op=ALU.add), reads=[big2], writes=[nblk])
            op("vector", lambda: V.tensor_tensor_scan(out=pend.t[:], data0=onesr.t[:], data1=nblk.t[:], initial=0.0,
                                                      op0=ALU.mult, op1=ALU.add), reads=[onesr, nblk], writes=[pend])
            op("vector", lambda: V.tensor_tensor(out=baserow.t[:], in0=pend.t[:], in1=nblk.t[:], op=ALU.subtract),
               reads=[pend, nblk], writes=[baserow])
            op("vector", lambda: V.tensor_scalar(out=baserow.t[:], in0=baserow.t[:], scalar1=float(RB), scalar2=None,
                                                 op0=ALU.mult), reads=[baserow], writes=[baserow])
            dma("sync", big.t[:], jv_d, writes=[big])
            op("vector", lambda: V.tensor_tensor(out=big2.t[:].rearrange("p (j e) -> p j e", j=NBLK),
                                                 in0=pend.t[:, :].unsqueeze(1).to_broadcast([128, NBLK, NE]),
                                                 in1=big.t[:].rearrange("p (j e) -> p j e", j=NBLK), op=ALU.is_le),
               reads=[pend, big], writes=[big2])
            op("vector", lambda: V.tensor_reduce(out=blke.t[:], in_=big2.t[:].rearrange("p (j e) -> p j e", j=NBLK),
                                                 axis=AX.X, op=ALU.add), reads=[big2], writes=[blke])
            op("vector", lambda: V.tensor_scalar(out=blke.t[:], in0=blke.t[:], scalar1=float(NE), scalar2=128.0,
                                                 op0=ALU.min, op1=ALU.mult), reads=[blke], writes=[blke])
            op("vector", lambda: V.tensor_scalar(out=blke.t[:], in0=blke.t[:], scalar1=pcol.t[:, 0:1], scalar2=None,
                                                 op0=ALU.add), reads=[blke, pcol], writes=[blke])
            op("vector", lambda: V.tensor_copy(out=widx.t[:], in_=blke.t[:]), reads=[blke], writes=[widx])
            r3 = lambda tl: tl.t[:].rearrange("p k e -> p (k e)")
            op("vector", lambda: V.tensor_tensor(out=rank_all.t[:], in0=rank_all.t[:],
                                                 in1=baserow.t[:, :].unsqueeze(1).to_broadcast([128, 32, NE]), op=ALU.add),
               reads=[rank_all, baserow], writes=[rank_all])
            op("vector", lambda: V.tensor_tensor(out=t1_all.t[:], in0=t1_all.t[:], in1=rank_all.t[:], op=ALU.mult),
               reads=[t1_all, rank_all], writes=[t1_all])
            op("vector", lambda: V.tensor_tensor(out=t2_all.t[:], in0=t2_all.t[:], in1=rank_all.t[:], op=ALU.mult),
               reads=[t2_all, rank_all], writes=[t2_all])
            dAf = tile(es, "dAf_all", [128, 32], F32)
            dBf = tile(es, "dBf_all", [128, 32], F32)
            op("vector", lambda: V.tensor_reduce(out=dAf.t[:], in_=t1_all.t[:], axis=AX.X, op=ALU.add), reads=[t1_all], writes=[dAf])
            op("vector", lambda: V.tensor_reduce(out=dBf.t[:], in_=t2_all.t[:], axis=AX.X, op=ALU.add), reads=[t2_all], writes=[dBf])
            op("vector", lambda: V.tensor_copy(out=dA_all.t[:], in_=dAf.t[:]), reads=[dAf], writes=[dA_all])
            op("vector", lambda: V.tensor_copy(out=dB_all.t[:], in_=dBf.t[:]), reads=[dBf], writes=[dB_all])
            for k in range(32):
                H2B = h2b[k % 2]
                dma("sync", H2B.t[:], h2_s[k * 128:(k + 1) * 128, :], reads=[dB["h2"]], writes=[H2B])
                dma("gpsimd", xg_s[:, :], H2B.t[:], reads=[H2B, dA_all], writes=[dB["xg"]],
                    out_off=bass.IndirectOffsetOnAxis(ap=dA_all.t[:, k:k + 1], axis=0))
                dma("gpsimd", xg_s[:, :], H2B.t[:], reads=[H2B, dB_all], writes=[dB["xg"]],
                    out_off=bass.IndirectOffsetOnAxis(ap=dB_all.t[:, k:k + 1], axis=0))
            T.barrier()
        if STOP_AFTER <= 4:
            T.barrier()
            return nc

        with ExitStack() as es:
            NW = 3
            wg = [tile(es, f"wg{i}", [128, 8 * 512], BF16) for i in range(NW)]
            wu = [tile(es, f"wu{i}", [128, 8 * 512], BF16) for i in range(NW)]
            wd = [tile(es, f"wd{i}", [128, 4 * D], BF16) for i in range(NW)]
            xr = [tile(es, f"xr{i}", [128, D], BF16) for i in range(3)]
            xT = [tile(es, f"xT{i}", [128, 8, RB], BF16) for i in range(2)]
            sg = [tile(es, f"sg{i}", [128, RB], F32) for i in range(2)]
            hid = [tile(es, f"hid{i}", [128, 4, RB], BF16) for i in range(2)]
            yt = [tile(es, f"yt{i}", [128, D], F32) for i in range(3)]
            ptr = [ptile(es, f"p4t{i}", [128, 1024], BF16) for i in range(2)]
            pG = [ptile(es, f"p4g{i}", [128, 512], F32) for i in range(2)]
            pU = [ptile(es, f"p4u{i}", [128, 512], F32) for i in range(2)]
            pY = [ptile(es, f"p4y{i}", [128, 512], F32) for i in range(2)]
            NRT = RB // 128
            bc_reg = nc.gpsimd.to_reg(NE * 128 - 1)
            for wt in wg + wu + wd:
                op("gpsimd", lambda: nc.gpsimd.memset(wt.t[:], 0.0), writes=[wt])

            def load_w(j):
                i = j % NW
                off = bass.IndirectOffsetOnAxis(ap=widx.t[:, j:j + 1], axis=0)
                kw = dict(bounds_check=bc_reg, oob_is_err=False)
                dma("gpsimd", wg[i].t[:], wg_d[:, :], reads=[widx], writes=[wg[i]], in_off=off, **kw)
                dma("gpsimd", wu[i].t[:], wu_d[:, :], reads=[widx], writes=[wu[i]], in_off=off, **kw)
                dma("gpsimd", wd[i].t[:], wd_d[:, :], reads=[widx], writes=[wd[i]], in_off=off, **kw)

            def stT(j):
                XT = xT[j % 2]
                for r in range(NRT):
                    c_ = j * NRT + r
                    XR, PT = xr[c_ % 3], ptr[c_ % 2]
                    row0 = j * RB + r * 128
                    dma("sync", XR.t[:], xg_s[row0:row0 + 128, :], reads=[dB["xg"]], writes=[XR])

                    def tr():
                        for kc in range(8):
                            i = nc.tensor.transpose(out=PT.t[:, kc * 128:(kc + 1) * 128], in_=XR.t[:, kc * 128:(kc + 1) * 128],
                                                    identity=identb.t[:])
                        return i
                    op("tensor", tr, reads=[XR, identb], writes=[PT])
                    op("scalar", lambda: nc.scalar.activation(out=XT.t[:, :, r * 128:(r + 1) * 128],
                                                              in_=PT.t[:].rearrange("p (kc t) -> p kc t", kc=8), func=AF.Copy),
                       reads=[PT], writes=[XT])

            def stH(j):
                XT, HID = xT[j % 2], hid[j % 2]
                WG = wg[j % NW].t[:].rearrange("p (kc f) -> p kc f", kc=8)
                WU = wu[j % NW].t[:].rearrange("p (kc f) -> p kc f", kc=8)
                for fc in range(4):
                    G, U, SG = pG[fc % 2], pU[fc % 2], sg[fc % 2]

                    def mmg():
                        for kc in range(8):
                            i = nc.tensor.matmul(G.t[:, 0:RB], lhsT=WG[:, kc, fc * 128:(fc + 1) * 128], rhs=XT.t[:, kc, :],
                                                 start=(kc == 0), stop=(kc == 7))
                        return i

                    def mmu():
                        for kc in range(8):
                            i = nc.tensor.matmul(U.t[:, 0:RB], lhsT=WU[:, kc, fc * 128:(fc + 1) * 128], rhs=XT.t[:, kc, :],
                                                 start=(kc == 0), stop=(kc == 7))
                        return i
                    op("tensor", mmg, reads=[wg[j % NW], XT], writes=[G])
                    op("tensor", mmu, reads=[wu[j % NW], XT], writes=[U])
                    op("scalar", lambda: nc.scalar.activation(out=SG.t[:], in_=G.t[:, 0:RB], func=AF.Silu), reads=[G], writes=[SG])
                    op("vector", lambda: nc.vector.tensor_tensor(out=HID.t[:, fc, :], in0=SG.t[:], in1=U.t[:, 0:RB], op=ALU.mult),
                       reads=[SG, U], writes=[HID])

            def stD(j):
                HID = hid[j % 2]
                WD = wd[j % NW].t[:].rearrange("p (fc d) -> p fc d", fc=4)
                for r in range(NRT):
                    YT = yt[(j * NRT + r) % 3]
                    for half in range(2):
                        PY = pY[half]

                        def mmy():
                            for fc in range(4):
                                i = nc.tensor.matmul(PY.t[:], lhsT=HID.t[:, fc, r * 128:(r + 1) * 128],
                                                     rhs=WD[:, fc, half * 512:(half + 1) * 512], start=(fc == 0), stop=(fc == 3))
                            return i
                        op("tensor", mmy, reads=[HID, wd[j % NW]], writes=[PY])
                        if half == 0:
                            op("scalar", lambda: nc.scalar.activation(out=YT.t[:, 0:512], in_=PY.t[:], func=AF.Copy), reads=[PY],
                               writes=[YT])
                        else:
                            op("vector", lambda: nc.vector.tensor_copy(out=YT.t[:, 512:1024], in_=PY.t[:]), reads=[PY], writes=[YT])
                    row0 = j * RB + r * 128
                    dma("scalar", y_s[row0:row0 + 128, :], YT.t[:], reads=[YT], writes=[dB["y"]])

            load_w(0)
            for step in range(NBLK + 2):
                if step < NBLK:
                    stT(step)
                if 0 <= step - 1 < NBLK:
                    stH(step - 1)
                if 0 <= step - 2 < NBLK:
                    stD(step - 2)
                if step + 1 < NBLK:
                    load_w(step + 1)
            T.barrier()
        if STOP_AFTER <= 5:
            T.barrier()
            return nc

        with ExitStack() as es:
            ya = [tile(es, f"ya{i}", [128, D], F32) for i in range(3)]
            yb = [tile(es, f"yb{i}", [128, D], F32) for i in range(3)]
            x1t = [tile(es, f"x5t{i}", [128, D], F32) for i in range(3)]
            mo_ = [tile(es, f"mo{i}", [128, D], F32) for i in range(2)]
            mo2_ = [tile(es, f"mo2{i}", [128, D], F32) for i in range(2)]
            ot = [tile(es, f"ot{i}", [128, D], F32) for i in range(2)]
            NB5 = 3

            def fetch(k):
                tok0 = k * 128
                dma("gpsimd", ya[k % NB5].t[:], y_s[:, :], reads=[dB["y"], dA_all], writes=[ya[k % NB5]],
                    in_off=bass.IndirectOffsetOnAxis(ap=dA_all.t[:, k:k + 1], axis=0))
                dma("gpsimd", yb[k % NB5].t[:], y_s[:, :], reads=[dB["y"], dB_all], writes=[yb[k % NB5]],
                    in_off=bass.IndirectOffsetOnAxis(ap=dB_all.t[:, k:k + 1], axis=0))
                dma("sync", x1t[k % NB5].t[:], x1_s[tok0:tok0 + 128, :], reads=[dB["x1"]], writes=[x1t[k % NB5]])
            fetch(0)
            fetch(1)
            for k in range(32):
                if k + 2 < 32:
                    fetch(k + 2)
                YA, YB, X1, OT = ya[k % NB5], yb[k % NB5], x1t[k % NB5], ot[k % 2]
                mo, mo2 = mo_[k % 2], mo2_[k % 2]
                tok0 = k * 128
                op("scalar", lambda: nc.scalar.activation(out=mo.t[:], in_=YA.t[:], func=AF.Copy, scale=wA_all.t[:, k:k + 1]),
                   reads=[YA, wA_all], writes=[mo])
                op("vector", lambda: nc.vector.scalar_tensor_tensor(out=mo2.t[:], in0=YB.t[:], scalar=wB_all.t[:, k:k + 1],
                                                                    in1=mo.t[:], op0=ALU.mult, op1=ALU.add),
                   reads=[YB, wB_all, mo], writes=[mo2])
                op("vector", lambda: nc.vector.tensor_tensor(out=mo.t[:], in0=mo2.t[:], in1=gtF.t[:], op=ALU.mult),
                   reads=[mo2, gtF], writes=[mo])
                op("gpsimd", lambda: nc.gpsimd.tensor_tensor(out=OT.t[:], in0=mo.t[:], in1=X1.t[:], op=ALU.add),
                   reads=[mo, X1], writes=[OT])
                dma("scalar", out_d[tok0:tok0 + 128, :], OT.t[:], reads=[OT], writes=[dB["out"]])
            T.barrier()
    return nc


_CONSTS = None


def host_consts(rel_bias):
    bf = ml_dtypes.bfloat16
    c = {}
    c["identb"] = np.eye(128, dtype=np.float32).astype(bf)
    c["identf"] = np.eye(128, dtype=np.float32)
    bd = np.zeros((128, 128), np.float32)
    bd[:64, :64] = 1.0 / 64
    bd[64:, 64:] = 1.0 / 64
    c["blkdiag"] = bd
    em = np.zeros((16, 16, 128), np.float32)
    for n in range(16):
        em[n, n, :] = 1.0
    c["emat"] = em.reshape(16, 16 * 128).astype(bf)
    j = np.arange(128)
    c["negtri"] = ((j[:, None] >= j[None, :]).astype(np.float32)).astype(bf)
    c["negones"] = (np.ones((128, 128), np.float32)).astype(bf)
    qi = np.arange(512)
    ms = np.zeros((128, 4, 512), np.float32)
    for jj in range(4):
        ms[:, jj, :] = ((jj * 128 + j[:, None]) < qi[None, :]).astype(np.float32)
    c["masks"] = ms.reshape(128, 4 * 512).astype(bf)
    c["ustrict"] = (j[:, None] < j[None, :]).astype(np.float32)
    c["onesf"] = np.ones((128, 128), np.float32)
    c["baserow"] = np.zeros((128, NE), np.float32)
    c["thr"] = np.broadcast_to((np.arange(32) * RB).astype(np.float32)[None, None, :], (128, NE, 32)).reshape(128, NE * 32).copy()
    c["jv"] = np.broadcast_to(np.arange(NBLK).astype(np.float32)[None, :, None], (128, NBLK, NE)).reshape(128, NBLK * NE).copy()
    c["pcol"] = np.arange(128, dtype=np.float32).reshape(128, 1)
    bt = np.zeros((H, 128, NOFF, 512), np.float32)
    for oi in range(NOFF):
        delta = -384 + 128 * oi
        d = delta + qi[None, :] - j[:, None]
        bk = rel_bucket_np(d)
        g = rel_bias[:, bk]
        g = np.where((d >= 0)[None], g, np.float32(-BIG))
        bt[:, :, oi, :] = g
    c["biast"] = bt.reshape(H, 128, NOFF * 512)
    c["blkind"] = (np.arange(S)[None, :] // 256 == np.arange(16)[:, None]).astype(np.float32).astype(bf)
    c["diagm"] = ((j[None, :] + j[:, None] - 127) <= 0).astype(np.float32)
    c["antii"] = np.eye(128, dtype=np.float32)[::-1].copy().astype(bf)
    c["bias31"] = np.broadcast_to(rel_bias[:, 31][None, :], (128, H)).astype(np.float32).copy()
    return c


def make_in_maps(x, c, w_ada, b_ada, g_mix, w_in, g_q, g_k, rel_bias, w_br_moba, w_br_sb, w_out,
                 g_ffn, w_rg, b_rg, w_re, b_re, w_gate, w_up, w_down, cores=range(8)):
    f = lambda a: np.ascontiguousarray(np.asarray(a, dtype=np.float32))
    x = f(x); c = f(c)
    consts = host_consts(f(rel_bias))
    gqk = np.stack([np.tile(f(g_q)[0], 2), np.tile(f(g_k)[0], 2)], axis=1)
    shared = {
        "w_ada": f(w_ada)[0], "b_ada": f(b_ada), "g_mix": f(g_mix), "w_in": f(w_in)[0], "gqk": np.ascontiguousarray(gqk),
        "w_br_moba": f(w_br_moba)[0], "w_br_sb": f(w_br_sb)[0], "w_out": f(w_out)[0], "g_ffn": f(g_ffn),
        "w_r": np.ascontiguousarray(np.concatenate([f(w_rg)[0], f(w_re)[0]], axis=1)),
        "b_r": np.ascontiguousarray(np.concatenate([f(b_rg), f(b_re)], axis=1)),
        "w_gate_r": np.ascontiguousarray(f(w_gate)[0].reshape(NE, 8, 128, 512).transpose(0, 2, 1, 3)).reshape(NE * 128, 8 * 512),
        "w_up_r": np.ascontiguousarray(f(w_up)[0].reshape(NE, 8, 128, 512).transpose(0, 2, 1, 3)).reshape(NE * 128, 8 * 512),
        "w_down_r": np.ascontiguousarray(f(w_down)[0].reshape(NE, 4, 128, D).transpose(0, 2, 1, 3)).reshape(NE * 128, 4 * D),
    }
    shared.update(consts)
    in_maps = []
    for b in cores:
        m = dict(shared)
        m["x"] = x[b]
        m["ccol"] = np.ascontiguousarray(c[b].reshape(8, 128).T)
        in_maps.append(m)
    return in_maps


def kernel(**inputs):
    in_maps = make_in_maps(**inputs)
    nc = build_program()
    res = run_bass_kernel_spmd(nc, in_maps, core_ids=list(range(8)))
    return np.stack([res.results[b]["out"] for b in range(8)], axis=0).astype(np.float32)
```
